# Optimizing a Trainium2 kernel written in Bass

```python
import math
import numpy as np
import jax
import jax.numpy as jnp
from jax import lax


D_MODEL = 1024
BATCH = 16
SEQ = 4096
DEPTH = 4

GRID_W = 64
CTX_LEN = 256
F32 = jnp.float32
NORM_EPS = 1e-6
ROPE_THETA = 10000.0
Q_BLOCK = 128
N_MOD = 6

SSD_HEADS = 16
SSD_HEAD_DIM = 64
SSD_INNER = SSD_HEADS * SSD_HEAD_DIM
SSD_GROUPS = 4
SSD_STATE = 128
SSD_BC = SSD_GROUPS * SSD_STATE
XBC_DIM = SSD_INNER + 2 * SSD_BC
SSD_CONV = 5
SSD_CHUNK = 128

NA_HEADS = 8
NA_HEAD_DIM = 64
NA_DIM = NA_HEADS * NA_HEAD_DIM
NA_ROWS = 8
NA_COLS = 16

EVEN_SIZES = (NA_DIM, NA_DIM, NA_DIM, SSD_INNER, XBC_DIM, SSD_HEADS, SSD_HEADS)
EVEN_IN = sum(EVEN_SIZES)
EVEN_MIX = NA_DIM + SSD_INNER

MLA_HEADS = 8
MLA_Q_LORA = 384
MLA_KV_LORA = 256
MLA_NOPE = 64
MLA_ROPE = 32
MLA_V = 64
MLA_SCALE = (MLA_NOPE + MLA_ROPE) ** -0.5

GQA_HEADS = 8
GQA_KV_HEADS = 2
GQA_GROUP = GQA_HEADS // GQA_KV_HEADS
GQA_HEAD_DIM = 64
GQA_SCALE = GQA_HEAD_DIM ** -0.5

ODD_Q_SIZES = (MLA_Q_LORA, GQA_HEADS * GQA_HEAD_DIM)
ODD_KV_SIZES = (MLA_KV_LORA, MLA_ROPE, GQA_KV_HEADS * GQA_HEAD_DIM, GQA_KV_HEADS * GQA_HEAD_DIM)
ODD_Q_COLS = sum(ODD_Q_SIZES)
ODD_IN = ODD_Q_COLS + sum(ODD_KV_SIZES)
ODD_MIX = MLA_HEADS * MLA_V + GQA_HEADS * GQA_HEAD_DIM

MOE_GROUPS = 4
MOE_EXPERTS_PER_GROUP = 8
MOE_EXPERTS = MOE_GROUPS * MOE_EXPERTS_PER_GROUP
MOE_TOPK = 2
MOE_HIDDEN = 512
MOE_BLOCK = 128

N_EVEN = (DEPTH + 1) // 2
N_ODD = DEPTH // 2
DEEPNORM_ALPHA = (2.0 * DEPTH) ** 0.25
DEEPNORM_BETA = (8.0 * DEPTH) ** -0.25

kernel_name = 'hybrid_ssd_natten_mla_gqa_hmoe_trunk'


def _split(t, sizes):
    return jnp.split(t, [int(s) for s in np.cumsum(sizes)[:-1]], axis=-1)


def layer_norm(x, g, b):
    xf = x.astype(F32)
    mu = jnp.mean(xf, -1, keepdims=True)
    var = jnp.mean(jnp.square(xf - mu), -1, keepdims=True)
    return ((xf - mu) * lax.rsqrt(var + NORM_EPS) * g + b).astype(x.dtype)


def rms_norm(x, g):
    xf = x.astype(F32)
    return (xf * lax.rsqrt(jnp.mean(jnp.square(xf), -1, keepdims=True) + NORM_EPS) * g).astype(x.dtype)


def modulate(x, shift, scale):
    return x * (1.0 + scale) + shift


def axial_rope(u):
    n, dim = u.shape[1], u.shape[-1]
    half = dim // 2
    t = jnp.arange(n, dtype=jnp.int32)
    inv_freq = jnp.power(ROPE_THETA, -jnp.arange(0, half, 2, dtype=F32) / half)

    def rotate(v, pos):
        ang = pos.astype(F32)[:, None] * inv_freq
        cos, sin = jnp.cos(ang)[:, None, :], jnp.sin(ang)[:, None, :]
        v1, v2 = jnp.split(v.astype(F32), 2, axis=-1)
        return jnp.concatenate([v1 * cos - v2 * sin, v1 * sin + v2 * cos], -1)

    out = jnp.concatenate([rotate(u[..., :half], t // GRID_W), rotate(u[..., half:], t % GRID_W)], -1)
    return out.astype(u.dtype)


def block_attention(q, k, v, scale):
    b, lq = q.shape[:2]
    nb = lq // Q_BLOCK
    qb = q.reshape((b, nb, Q_BLOCK) + q.shape[2:]).swapaxes(0, 1)

    def one(qi):
        s = jnp.einsum('bqhrd,bkhd->bhrqk', qi, k).astype(F32) * scale
        p = jax.nn.softmax(s, axis=-1).astype(v.dtype)
        return jnp.einsum('bhrqk,bkhe->bqhre', p, v)

    out = lax.map(one, qb)
    return out.swapaxes(0, 1).reshape((b, lq) + out.shape[3:])


def neighbourhood_attention(q, k, v, k_ctx, v_ctx, rpb, rows):
    b, n, heads, dh = q.shape
    kh, kw = min(NA_ROWS, rows), NA_COLS
    scale = dh ** -0.5
    col_start = np.clip(np.arange(GRID_W) - kw // 2, 0, GRID_W - kw)
    col_idx = col_start[:, None] + np.arange(kw)[None, :]
    dc = col_idx - np.arange(GRID_W)[:, None] + (NA_COLS - 1)
    bias_cols = rpb[:, :, dc]
    qg = q.reshape(b, rows, GRID_W, heads, dh).swapaxes(0, 1)
    kg = k.reshape(b, rows, GRID_W, heads, dh)
    vg = v.reshape(b, rows, GRID_W, heads, dh)

    def row_block(args):
        q_row, r = args
        r0 = jnp.clip(r - kh // 2, 0, rows - kh)
        k_win = lax.dynamic_slice_in_dim(kg, r0, kh, axis=1)[:, :, col_idx]
        v_win = lax.dynamic_slice_in_dim(vg, r0, kh, axis=1)[:, :, col_idx]
        dr = r0 + jnp.arange(kh) - r + (NA_ROWS - 1)
        bias = jnp.take(bias_cols, dr, axis=1).transpose(0, 2, 1, 3).astype(F32)
        s_loc = jnp.einsum('bjhd,bkjwhd->bhjkw', q_row, k_win).astype(F32) * scale + bias
        s_ctx = jnp.einsum('bjhd,bchd->bhjc', q_row, k_ctx).astype(F32) * scale
        s = jnp.concatenate([s_loc.reshape(b, heads, GRID_W, kh * kw), s_ctx], -1)
        p = jax.nn.softmax(s, axis=-1).astype(v.dtype)
        p_loc = p[..., :kh * kw].reshape(b, heads, GRID_W, kh, kw)
        return (jnp.einsum('bhjkw,bkjwhd->bjhd', p_loc, v_win)
                + jnp.einsum('bhjc,bchd->bjhd', p[..., kh * kw:], v_ctx))

    out = lax.map(row_block, (qg, jnp.arange(rows, dtype=jnp.int32)))
    return out.swapaxes(0, 1).reshape(b, n, heads * dh)


def depthwise_conv_centred(u, w, bias):
    taps, ch = w.shape
    out = lax.conv_general_dilated(u, w[:, None, :].astype(u.dtype), window_strides=(1,),
                                   padding=[(taps // 2, taps // 2)],
                                   dimension_numbers=('NWC', 'WIO', 'NWC'), feature_group_count=ch)
    return out + bias


def ssd_chunk_scan(xdt, a, bm, cm, h0):
    b, seq, heads, hp = xdt.shape
    groups, ns = bm.shape[2], bm.shape[3]
    rep = heads // groups
    q = SSD_CHUNK
    nc = seq // q
    x_c = xdt.reshape(b, nc, q, groups, rep, hp)
    a_cs = jnp.cumsum(a.astype(F32).reshape(b, nc, q, groups, rep), axis=2)
    b_c = bm.reshape(b, nc, q, groups, ns)
    c_c = cm.reshape(b, nc, q, groups, ns)
    seg = a_cs[:, :, :, None] - a_cs[:, :, None, :]
    lower = np.tril(np.ones((q, q), dtype=bool))[:, :, None, None]
    decay = jnp.exp(jnp.where(lower, seg, -jnp.inf))
    cb = jnp.einsum('bclgn,bcsgn->bclsg', c_c, b_c)
    y_diag = jnp.einsum('bclsgr,bcsgrp->bclgrp', decay * cb[..., None], x_c)
    decay_to_end = jnp.exp(a_cs[:, :, -1:] - a_cs)
    s_chunk = jnp.einsum('bclgn,bclgr,bclgrp->bcgrpn', b_c, decay_to_end, x_c)
    chunk_decay = jnp.exp(a_cs[:, :, -1])

    def step(h, inp):
        dec, s = inp
        return dec[..., None, None] * h + s, h

    h_last, h_in = lax.scan(step, h0, (jnp.moveaxis(chunk_decay, 1, 0), jnp.moveaxis(s_chunk, 1, 0)))
    h_in = jnp.moveaxis(h_in, 0, 1)
    y_off = jnp.einsum('bclgn,bcgrpn,bclgr->bclgrp', c_c, h_in, jnp.exp(a_cs))
    return (y_diag + y_off).reshape(b, seq, heads, hp), h_last


def ssd_direction(xs, bm, cm, dt, a_coef, h0, reverse):
    order = (lambda t: jnp.flip(t, axis=1)) if reverse else (lambda t: t)
    y, h_last = ssd_chunk_scan(order(xs * dt[..., None]), order(dt * a_coef), order(bm), order(cm), h0)
    return order(y).astype(xs.dtype), h_last


def even_mixer(h, hc, rows, w_in, w_out, conv_w, conv_b, dt_bias, a_log, d_skip, gnorm_w, rpb):
    b = h.shape[0]
    q, k, v, z, xbc, dtf, dtb = _split(h @ w_in, EVEN_SIZES)
    qc, kc, vc, zc, xbcc, dtfc, dtbc = _split(hc @ w_in, EVEN_SIZES)
    heads = lambda t: t.reshape(t.shape[0], t.shape[1], NA_HEADS, NA_HEAD_DIM)
    q, k, v, qc, kc, vc = heads(q), heads(k), heads(v), heads(qc), heads(kc), heads(vc)
    na_lat = neighbourhood_attention(q, k, v, kc, vc, rpb, rows)
    na_ctx = block_attention(qc[:, :, :, None], kc, vc, NA_HEAD_DIM ** -0.5).reshape(b, hc.shape[1], NA_DIM)

    def ssd_branch(xbc_, z_, dts, h0s):
        u = jax.nn.silu(depthwise_conv_centred(xbc_, conv_w, conv_b))
        xs, bm, cm = _split(u, (SSD_INNER, SSD_BC, SSD_BC))
        nb, n = xs.shape[:2]
        xs = xs.reshape(nb, n, SSD_HEADS, SSD_HEAD_DIM)
        bm = bm.reshape(nb, n, SSD_GROUPS, SSD_STATE)
        cm = cm.reshape(nb, n, SSD_GROUPS, SSD_STATE)
        y = d_skip[:, None] * xs
        finals = []
        for d in range(2):
            dt = jax.nn.softplus(dts[d].astype(F32) + dt_bias[d])
            yd, hd = ssd_direction(xs, bm, cm, dt, -jnp.exp(a_log[d].astype(F32)), h0s[d], d == 1)
            y = y + yd
            finals.append(hd)
        y = y.reshape(nb, n, SSD_INNER) * jax.nn.silu(z_)
        return rms_norm(y, gnorm_w), finals

    h0 = jnp.zeros((b, SSD_GROUPS, SSD_HEADS // SSD_GROUPS, SSD_HEAD_DIM, SSD_STATE), F32)
    ssd_ctx, ctx_states = ssd_branch(xbcc, zc, (dtfc, dtbc), (h0, h0))
    ssd_lat, _ = ssd_branch(xbc, z, (dtf, dtb), ctx_states)
    y_lat = jnp.concatenate([na_lat, ssd_lat], -1) @ w_out
    y_ctx = jnp.concatenate([na_ctx, ssd_ctx], -1) @ w_out
    return y_lat, y_ctx


def mla_queries(cq, norm_w, w_uq, rotary):
    b, n, _ = cq.shape
    q = (rms_norm(cq, norm_w) @ w_uq).reshape(b, n, MLA_HEADS, MLA_NOPE + MLA_ROPE)
    q_nope, q_rot = q[..., :MLA_NOPE], q[..., MLA_NOPE:]
    if rotary:
        q_rot = axial_rope(q_rot)
    return jnp.concatenate([q_nope, q_rot], -1)


def mla_keys_values(ckv, k_rot, norm_w, w_ukv, rotary):
    b, n, _ = ckv.shape
    kv = (rms_norm(ckv, norm_w) @ w_ukv).reshape(b, n, MLA_HEADS, MLA_NOPE + MLA_V)
    k_nope, v = kv[..., :MLA_NOPE], kv[..., MLA_NOPE:]
    k_rot = k_rot[:, :, None, :]
    if rotary:
        k_rot = axial_rope(k_rot)
    k = jnp.concatenate([k_nope, jnp.broadcast_to(k_rot, (b, n, MLA_HEADS, MLA_ROPE))], -1)
    return k, v


def odd_mixer(h, hc, w_in, w_out, mla_q_norm, mla_kv_norm, w_uq, w_ukv, gqa_q_norm, gqa_k_norm, ctx_queries):
    cq, gq, ckv, krot, gk, gv = _split(h @ w_in, ODD_Q_SIZES + ODD_KV_SIZES)
    if ctx_queries:
        cq_c, gq_c, ckv_c, krot_c, gk_c, gv_c = _split(hc @ w_in, ODD_Q_SIZES + ODD_KV_SIZES)
    else:
        ckv_c, krot_c, gk_c, gv_c = _split(hc @ w_in[:, ODD_Q_COLS:], ODD_KV_SIZES)
    q_heads = lambda t: t.reshape(t.shape[0], t.shape[1], GQA_HEADS, GQA_HEAD_DIM)
    kv_heads = lambda t: t.reshape(t.shape[0], t.shape[1], GQA_KV_HEADS, GQA_HEAD_DIM)
    q_groups = lambda t: t.reshape(t.shape[0], t.shape[1], GQA_KV_HEADS, GQA_GROUP, GQA_HEAD_DIM)
    mk_c, mv_c = mla_keys_values(ckv_c, krot_c, mla_kv_norm, w_ukv, rotary=False)
    gk_c = rms_norm(kv_heads(gk_c), gqa_k_norm)
    gv_c = kv_heads(gv_c)
    mk, mv = mla_keys_values(ckv, krot, mla_kv_norm, w_ukv, rotary=True)
    mq = mla_queries(cq, mla_q_norm, w_uq, rotary=True)
    gq = q_groups(axial_rope(rms_norm(q_heads(gq), gqa_q_norm)))
    gk = axial_rope(rms_norm(kv_heads(gk), gqa_k_norm))
    gv = kv_heads(gv)
    cat = lambda first, second: jnp.concatenate([first, second], axis=1)

    def mix(mq_, gq_, mk_, mv_, gk_, gv_):
        bsz, m = mq_.shape[:2]
        o_mla = block_attention(mq_[:, :, :, None], mk_, mv_, MLA_SCALE).reshape(bsz, m, MLA_HEADS * MLA_V)
        o_gqa = block_attention(gq_, gk_, gv_, GQA_SCALE).reshape(bsz, m, GQA_HEADS * GQA_HEAD_DIM)
        return jnp.concatenate([o_mla, o_gqa], -1) @ w_out

    y_lat = mix(mq, gq, cat(mk_c, mk), cat(mv_c, mv), cat(gk_c, gk), cat(gv_c, gv))
    if not ctx_queries:
        return y_lat, None
    mq_c = mla_queries(cq_c, mla_q_norm, w_uq, rotary=False)
    gq_c = q_groups(rms_norm(q_heads(gq_c), gqa_q_norm))
    y_ctx = mix(mq_c, gq_c, mk_c, mv_c, gk_c, gv_c)
    return y_lat, y_ctx


def grouped_expert_mlp(h, expert, gate, w1, w3, w2):
    t_tok, dm = h.shape
    n_exp = w1.shape[0]
    n_assign = t_tok * MOE_TOPK
    flat_e = expert.reshape(n_assign).astype(jnp.int32)
    flat_tok = jnp.repeat(jnp.arange(t_tok, dtype=jnp.int32), MOE_TOPK)
    flat_w = gate.reshape(n_assign)
    order = jnp.argsort(flat_e)
    se = flat_e[order]
    counts = jnp.bincount(flat_e, length=n_exp)
    starts = jnp.cumsum(counts) - counts
    pcounts = (counts + MOE_BLOCK - 1) // MOE_BLOCK * MOE_BLOCK
    pends = jnp.cumsum(pcounts)
    pstarts = pends - pcounts
    dest = pstarts[se] + (jnp.arange(n_assign, dtype=jnp.int32) - starts[se])
    nb = (n_assign + n_exp * (MOE_BLOCK - 1)) // MOE_BLOCK
    slot_tok = jnp.full((nb * MOE_BLOCK,), t_tok, jnp.int32).at[dest].set(flat_tok[order])
    slot_w = jnp.zeros((nb * MOE_BLOCK,), h.dtype).at[dest].set(flat_w[order].astype(h.dtype))
    blk_e = jnp.minimum(jnp.searchsorted(pends, jnp.arange(nb, dtype=jnp.int32) * MOE_BLOCK, side='right'),
                        n_exp - 1)
    hp = jnp.concatenate([h, jnp.zeros((1, dm), h.dtype)], 0)
    xb = hp[slot_tok].reshape(nb, MOE_BLOCK, dm)

    def expert_block(args):
        xi, e = args
        return (jax.nn.silu(xi @ w1[e]) * (xi @ w3[e])) @ w2[e]

    yb = lax.map(expert_block, (xb, blk_e)).reshape(nb * MOE_BLOCK, dm)
    out = jnp.zeros((t_tok + 1, dm), h.dtype).at[slot_tok].add(yb * slot_w[:, None])
    return out[:t_tok]


def hier_moe(h, wg, bg, we, be, w1, w3, w2):
    t_tok = h.shape[0]
    g_prob = jax.nn.softmax((h @ wg).astype(F32) + bg, axis=-1)
    g_gate, g_sel = lax.top_k(g_prob, 1)
    e_logits = ((h @ we).astype(F32) + be).reshape(t_tok, MOE_GROUPS, MOE_EXPERTS_PER_GROUP)
    e_in = jnp.take_along_axis(e_logits, g_sel[:, :, None], axis=1)[:, 0]
    e_top, e_idx = lax.top_k(e_in, MOE_TOPK)
    gate = g_gate * jax.nn.softmax(e_top, axis=-1)
    expert = g_sel * MOE_EXPERTS_PER_GROUP + e_idx
    return grouped_expert_mlp(h, expert, gate, w1, w3, w2)


def setup_inputs(seed: int = 0) -> dict:
    key = jax.random.key(seed)
    ks = iter(jax.random.split(key, 40))
    nrm = lambda shape, scale: jax.random.normal(next(ks), shape, F32) * scale
    dm = D_MODEL
    dt0 = jnp.exp(jax.random.uniform(next(ks), (N_EVEN, 2, SSD_HEADS), F32, math.log(1e-3), math.log(1e-1)))
    a0 = jax.random.uniform(next(ks), (N_EVEN, 2, SSD_HEADS), F32, 1.0, 16.0)
    return {
        'x': nrm((BATCH, SEQ, dm), 1.0),
        'c': nrm((BATCH, dm), 1.0),
        'ctx': nrm((BATCH, CTX_LEN, dm), 1.0),
        'c_ctx': nrm((dm,), 1.0),
        'ada_w': nrm((DEPTH, dm, N_MOD * dm), 0.5 * dm ** -0.5),
        'ada_b': nrm((DEPTH, N_MOD * dm), 0.02),
        'ln_g': 1.0 + nrm((DEPTH, 2, dm), 0.02),
        'ln_b': nrm((DEPTH, 2, dm), 0.02),
        'e_w_in': nrm((N_EVEN, dm, EVEN_IN), dm ** -0.5),
        'e_w_out': nrm((N_EVEN, EVEN_MIX, dm), DEEPNORM_BETA * EVEN_MIX ** -0.5),
        'e_conv_w': nrm((N_EVEN, SSD_CONV, XBC_DIM), SSD_CONV ** -0.5),
        'e_conv_b': nrm((N_EVEN, XBC_DIM), 0.01),
        'e_dt_bias': dt0 + jnp.log(-jnp.expm1(-dt0)),
        'e_a_log': jnp.log(a0),
        'e_d_skip': 1.0 + nrm((N_EVEN, SSD_HEADS), 0.02),
        'e_gnorm_w': 1.0 + nrm((N_EVEN, SSD_INNER), 0.02),
        'e_rpb': nrm((N_EVEN, NA_HEADS, 2 * NA_ROWS - 1, 2 * NA_COLS - 1), 0.02),
        'o_w_in': nrm((N_ODD, dm, ODD_IN), dm ** -0.5),
        'o_w_out': nrm((N_ODD, ODD_MIX, dm), DEEPNORM_BETA * ODD_MIX ** -0.5),
        'o_mla_q_norm': 1.0 + nrm((N_ODD, MLA_Q_LORA), 0.02),
        'o_mla_kv_norm': 1.0 + nrm((N_ODD, MLA_KV_LORA), 0.02),
        'o_w_uq': nrm((N_ODD, MLA_Q_LORA, MLA_HEADS * (MLA_NOPE + MLA_ROPE)), MLA_Q_LORA ** -0.5),
        'o_w_ukv': nrm((N_ODD, MLA_KV_LORA, MLA_HEADS * (MLA_NOPE + MLA_V)), MLA_KV_LORA ** -0.5),
        'o_gqa_q_norm': 1.0 + nrm((N_ODD, GQA_HEAD_DIM), 0.02),
        'o_gqa_k_norm': 1.0 + nrm((N_ODD, GQA_HEAD_DIM), 0.02),
        'moe_wg': nrm((DEPTH, dm, MOE_GROUPS), dm ** -0.5),
        'moe_bg': nrm((DEPTH, MOE_GROUPS), 0.01),
        'moe_we': nrm((DEPTH, dm, MOE_EXPERTS), dm ** -0.5),
        'moe_be': nrm((DEPTH, MOE_EXPERTS), 0.01),
        'moe_w1': nrm((DEPTH, MOE_EXPERTS, dm, MOE_HIDDEN), dm ** -0.5),
        'moe_w3': nrm((DEPTH, MOE_EXPERTS, dm, MOE_HIDDEN), dm ** -0.5),
        'moe_w2': nrm((DEPTH, MOE_EXPERTS, MOE_HIDDEN, dm), DEEPNORM_BETA * MOE_HIDDEN ** -0.5),
    }


def reference(x, c, ctx, c_ctx, ada_w, ada_b, ln_g, ln_b,
              e_w_in, e_w_out, e_conv_w, e_conv_b, e_dt_bias, e_a_log, e_d_skip, e_gnorm_w, e_rpb,
              o_w_in, o_w_out, o_mla_q_norm, o_mla_kv_norm, o_w_uq, o_w_ukv, o_gqa_q_norm, o_gqa_k_norm,
              moe_wg, moe_bg, moe_we, moe_be, moe_w1, moe_w3, moe_w2):
    rows = x.shape[1] // GRID_W
    s_c = jax.nn.silu(c)
    s_cc = jax.nn.silu(c_ctx)
    xc = ctx
    for layer in range(DEPTH):
        ctx_needed = layer < DEPTH - 1
        i = layer // 2
        mod = jnp.split((s_c @ ada_w[layer] + ada_b[layer])[:, None, :], N_MOD, axis=-1)
        mod_c = jnp.split(s_cc @ ada_w[layer] + ada_b[layer], N_MOD, axis=-1)
        h = modulate(x, mod[0], mod[1])
        hc = modulate(xc, mod_c[0], mod_c[1])
        if layer % 2 == 0:
            y, yc = even_mixer(h, hc, rows, e_w_in[i], e_w_out[i], e_conv_w[i], e_conv_b[i], e_dt_bias[i],
                               e_a_log[i], e_d_skip[i], e_gnorm_w[i], e_rpb[i])
        else:
            y, yc = odd_mixer(h, hc, o_w_in[i], o_w_out[i], o_mla_q_norm[i], o_mla_kv_norm[i], o_w_uq[i],
                              o_w_ukv[i], o_gqa_q_norm[i], o_gqa_k_norm[i], ctx_needed)
        x = layer_norm(DEEPNORM_ALPHA * x + mod[2] * y, ln_g[layer, 0], ln_b[layer, 0])
        h = modulate(x, mod[3], mod[4])
        if ctx_needed:
            xc = layer_norm(DEEPNORM_ALPHA * xc + mod_c[2] * yc, ln_g[layer, 0], ln_b[layer, 0])
            hc = modulate(xc, mod_c[3], mod_c[4])
            n_ctx = hc.shape[0] * hc.shape[1]
            tokens = jnp.concatenate([hc.reshape(n_ctx, D_MODEL), h.reshape(-1, D_MODEL)], 0)
            f = hier_moe(tokens, moe_wg[layer], moe_bg[layer], moe_we[layer], moe_be[layer],
                         moe_w1[layer], moe_w3[layer], moe_w2[layer])
            xc = layer_norm(DEEPNORM_ALPHA * xc + mod_c[5] * f[:n_ctx].reshape(hc.shape),
                            ln_g[layer, 1], ln_b[layer, 1])
            f = f[n_ctx:].reshape(h.shape)
        else:
            f = hier_moe(h.reshape(-1, D_MODEL), moe_wg[layer], moe_bg[layer], moe_we[layer], moe_be[layer],
                         moe_w1[layer], moe_w3[layer], moe_w2[layer]).reshape(h.shape)
        x = layer_norm(DEEPNORM_ALPHA * x + mod[5] * f, ln_g[layer, 1], ln_b[layer, 1])
    return x
```

```python
import contextlib
import numpy as np
import concourse.bass as bass
import concourse.mybir as mybir
from concourse.bass_utils import run_bass_kernel_spmd

F32 = mybir.dt.float32
BF16 = mybir.dt.bfloat16
I32 = mybir.dt.int32
AF = mybir.ActivationFunctionType
ALU = mybir.AluOpType
AX = mybir.AxisListType

ENGS = ['pe', 'act', 'dve', 'pool', 'sp']
DMAQ = ('sp', 'act', 'pool')
NDS = 10

D = 1024
NB = 2
LC = 256
LL = 4096
LT = LC + LL
NT = LT // 128
DEPTH = 4
ALPHA = (2.0 * DEPTH) ** 0.25
EPS = 1e-6


class _Op:
    __slots__ = ('eng', 'fn', 'waits', 'dma', 'slot', 'val', 'sig', 'prev')

    def __init__(self, eng, fn, dma):
        self.eng = eng
        self.fn = fn
        self.dma = dma
        self.waits = []
        self.slot = None
        self.val = None
        self.sig = False
        self.prev = None


class Sched:
    def __init__(self, nc):
        self.nc = nc
        self.ops = {e: [] for e in ENGS}
        self.lastw = {}
        self.reads = {}
        self.dcnt = {e: [0] * NDS for e in DMAQ}
        self.dlast = {e: [None] * NDS for e in DMAQ}
        self.di = {e: 0 for e in DMAQ}
        self.lastc = {e: None for e in ENGS}

    def _add(self, eng, fn, reads, writes, dma):
        op = _Op(eng, fn, dma)
        if dma:
            s = self.di[eng] % NDS
            self.di[eng] += 1
            self.dcnt[eng][s] += 16
            op.slot = s
            op.val = self.dcnt[eng][s]
            op.sig = True
            op.prev = self.dlast[eng][s]
            self.dlast[eng][s] = op
        else:
            self.lastc[eng] = op
        seen = set()
        for k in reads:
            w = self.lastw.get(k)
            if w is not None and id(w) not in seen:
                seen.add(id(w))
                self._dep(op, w, 'raw')
        for k in writes:
            w = self.lastw.get(k)
            if w is not None and id(w) not in seen:
                seen.add(id(w))
                self._dep(op, w, 'waw')
            for r in self.reads.get(k, {}).values():
                if id(r) not in seen:
                    seen.add(id(r))
                    self._dep(op, r, 'war')
        for k in writes:
            self.lastw[k] = op
            self.reads[k] = {}
        for k in reads:
            rk = self.reads.setdefault(k, {})
            rk[(eng, id(op)) if dma else eng] = op
        self.ops[eng].append(op)
        return op

    def _dep(self, op, d, kind):
        if d is op:
            return
        if (not d.dma) and (not op.dma) and d.eng == op.eng and (kind != 'raw' or op.eng == 'pe'):
            return
        d.sig = True
        op.waits.append(d)

    def op(self, eng, fn, reads=(), writes=()):
        return self._add(eng, fn, reads, writes, False)

    def dma(self, eng, fn, reads=(), writes=()):
        return self._add(eng, fn, reads, writes, True)

    def barrier(self):
        marks = []
        for e in ENGS:
            if self.lastc[e] is not None:
                self.lastc[e].sig = True
                marks.append(self.lastc[e])
        for q in DMAQ:
            for s in range(NDS):
                if self.dlast[q][s] is not None:
                    marks.append(self.dlast[q][s])
        for e in ENGS:
            op = _Op(e, None, False)
            op.waits = [m for m in marks if not (m.eng == e and not m.dma)]
            self.ops[e].append(op)
        self.lastw = {}
        self.reads = {}

    def emit(self):
        nc = self.nc
        self.barrier()
        with contextlib.ExitStack() as st:
            esem = {e: st.enter_context(nc.semaphore('s_' + e)) for e in ENGS}
            dsem = {e: [st.enter_context(nc.semaphore('d_%s_%d' % (e, i))) for i in range(NDS)] for e in DMAQ}
            for e in ENGS:
                cnt = 0
                for op in self.ops[e]:
                    if (not op.dma) and op.sig:
                        cnt += 1
                        op.val = cnt
            block = st.enter_context(nc.Block())

            def semof(d):
                return dsem[d.eng][d.slot] if d.dma else esem[d.eng]

            def run(e, eng):
                waited = {}
                for op in self.ops[e]:
                    ws = {}
                    lst = list(op.waits)
                    if op.dma and op.prev is not None:
                        lst.append(op.prev)
                    for d in lst:
                        s = semof(d)
                        if ws.get(id(s), (0, None))[0] < d.val:
                            ws[id(s)] = (d.val, s)
                    for key, (v, s) in ws.items():
                        if waited.get(key, 0) < v:
                            eng.wait_ge(s, v)
                            waited[key] = v
                    if op.fn is None:
                        continue
                    ins = op.fn(eng)
                    if op.sig:
                        ins.then_inc(semof(op), 16 if op.dma else 1)

            @block.sync
            def _(eng):
                run('sp', eng)

            @block.tensor
            def _(eng):
                run('pe', eng)

            @block.scalar
            def _(eng):
                run('act', eng)

            @block.vector
            def _(eng):
                run('dve', eng)

            @block.gpsimd
            def _(eng):
                run('pool', eng)


class KB:
    def __init__(self, nc, st):
        self.nc = nc
        self.S = Sched(nc)
        self.AR = 50688
        self.arena = st.enter_context(nc.sbuf_tensor("arena", [128, self.AR], F32))
        self.banks = [st.enter_context(nc.psum_tensor("bank%d" % i, [128, 512], F32)) for i in range(8)]
        self.off = 0
        self.ptop = self.AR
        self.uid = 0

    def phase(self):
        self.S.barrier()
        self.off = 0

    def _view(self, a, n32, shape, dtype):
        v = self.arena[:, a:a + n32]
        if dtype == BF16:
            v = v.bitcast(BF16)
        elif dtype == I32:
            v = v.bitcast(I32)
        if len(shape) == 2:
            v = v.rearrange("p (a b) -> p a b", a=shape[0])
        elif len(shape) == 3:
            v = v.rearrange("p (a b c) -> p a b c", a=shape[0], b=shape[1])
        return v

    def sb(self, shape, dtype=F32, persist=False):
        n = int(np.prod(shape))
        n32 = n if dtype != BF16 else (n + 1) // 2
        if persist:
            self.ptop -= n32
            a = self.ptop
        else:
            a = self.off
            self.off += n32
        assert self.off <= self.ptop, "SBUF arena overflow %d > %d" % (self.off, self.ptop)
        self.uid += 1
        return self._view(a, n32, shape, dtype), 'sb%d' % self.uid

    def bank(self, i, shape=None, dtype=F32):
        v = self.banks[i][:]
        if dtype == BF16:
            v = v.bitcast(BF16)
        if shape is not None and len(shape) == 2:
            v = v[:, 0:shape[0] * shape[1]].rearrange("p (a b) -> p a b", a=shape[0])
        elif shape is not None and len(shape) == 1:
            v = v[:, 0:shape[0]]
        return v, 'bank%d' % i

    def dma(self, q, out, in_, r=(), w=()):
        return self.S.dma(q, lambda e: e.dma_start(out=out, in_=in_), reads=r, writes=w)

    def mm(self, out, lhsT, rhs, start, stop, r=(), w=()):
        return self.S.op('pe', lambda e: e.matmul(out, lhsT=lhsT, rhs=rhs, start=start, stop=stop), reads=r, writes=w)

    def tr(self, out, in_, ident, r=(), w=()):
        return self.S.op('pe', lambda e: e.transpose(out=out, in_=in_, identity=ident), reads=r, writes=w)

    def act(self, out, in_, func, bias=None, scale=1.0, accum=None, r=(), w=(), eng='act'):
        def f(e):
            kw = {}
            if bias is not None:
                kw['bias'] = bias
            if accum is not None:
                kw['accum_out'] = accum
            return e.activation(out=out, in_=in_, func=func, scale=scale, **kw)
        return self.S.op('act', f, reads=r, writes=w)

    def ts(self, eng, out, in0, s1, s2, op0, op1=None, accum=None, r=(), w=()):
        def f(e):
            kw = {}
            if accum is not None:
                kw['accum_out'] = accum
            if op1 is None:
                return e.tensor_scalar(out, in0, s1, None, op0, **kw)
            return e.tensor_scalar(out, in0, s1, s2, op0, op1, **kw)
        return self.S.op(eng, f, reads=r, writes=w)

    def tt(self, eng, out, in0, in1, op, r=(), w=()):
        return self.S.op(eng, lambda e: e.tensor_tensor(out, in0, in1, op), reads=r, writes=w)

    def stt(self, out, in0, scalar, in1, op0, op1, r=(), w=()):
        return self.S.op('dve', lambda e: e.scalar_tensor_tensor(out, in0, scalar, in1, op0, op1), reads=r, writes=w)

    def cp(self, eng, out, in_, r=(), w=()):
        if eng == 'act':
            return self.S.op('act', lambda e: e.copy(out=out, in_=in_), reads=r, writes=w)
        return self.S.op(eng, lambda e: e.tensor_copy(out=out, in_=in_), reads=r, writes=w)

    def red(self, out, in_, op, r=(), w=(), axis=None):
        ax = AX.X if axis is None else axis
        return self.S.op('dve', lambda e: e.tensor_reduce(out, in_, ax, op), reads=r, writes=w)

    def recip(self, out, in_, r=(), w=()):
        return self.S.op('dve', lambda e: e.reciprocal(out, in_), reads=r, writes=w)

    def memset(self, eng, ap, c, w=()):
        return self.S.op(eng, lambda e: e.memset(ap, c), writes=w)


def P(name):
    return name


class LSel:
    def __init__(self, ap, base):
        self.ap = ap
        self.base = base

    def __getitem__(self, key):
        if isinstance(key, tuple):
            return self.ap[(key[0] - self.base,) + tuple(key[1:])]
        return self.ap[key - self.base]


PER_LAYER = ('ada_w', 'ada_b', 'ln_g', 'ln_b', 'moe_wr', 'moe_br', 'moe_w1', 'moe_w3', 'moe_w2')
PER_PAIR = ('o_w_in', 'o_w_out', 'o_mla_q_norm', 'o_mla_kv_norm', 'o_w_uq', 'o_w_ukv', 'o_gqa_q_norm', 'o_gqa_k_norm',
            'e_w_in', 'e_w_out', 'e_conv_w', 'e_conv_b', 'e_dt_bias', 'e_a_log', 'e_d_skip', 'e_gnorm_w', 'nabias')


class Prog:
    def __init__(self, cfg):
        self.cfg = cfg
        nc = bass.Bass("TRN2", target_bir_lowering=False)
        self.nc = nc
        self.st = contextlib.ExitStack()
        self.kb = KB(nc, self.st)
        self.din = {}
        self.uid = 0

    def inp(self, name, shape, dtype=F32):
        only = self.cfg.get('only_layer')
        shape = list(shape)
        base = None
        if only is not None and name in PER_LAYER:
            shape[0] = 1
            base = only
        elif only is not None and name in PER_PAIR:
            if (name[0] == 'o') != (only % 2 == 1):
                return None
            shape[0] = 1
            base = only // 2
        elif only is not None and name in ('rope',) and only % 2 == 0:
            return None
        elif only is not None and name in ('tri',) and only % 2 == 1:
            return None
        t = self.nc.dram_tensor(name, shape, dtype, kind="ExternalInput").ap()
        self.din[name] = t
        return LSel(t, base) if base is not None else t

    def scratch(self, name, shape, dtype=F32, out=False):
        return self.nc.dram_tensor(name, list(shape), dtype, kind="ExternalOutput" if out else "Internal").ap()

    def prologue(self):
        kb = self.kb
        cin = self.inp("cin", [4, D])
        ada_w = self.inp("ada_w", [DEPTH, D, 6 * D])
        ada_b = self.inp("ada_b", [DEPTH, 6 * D])
        identd = self.inp("ident", [128, 128])
        self.modd = self.scratch("modd", [DEPTH, 4, 6 * D])
        self.identf, kif = kb.sb([128], F32, persist=True)
        self.identb, kib = kb.sb([128], BF16, persist=True)
        self.modT, kmt = kb.sb([DEPTH * 4, 48], F32, persist=True)
        self.k_modT = kmt
        kb.dma('sp', self.identf, identd, w=[kif])
        kb.cp('dve', self.identb, self.identf, r=[kif], w=[kib])
        scr, kscr = kb.sb([4, 8])
        sc, ksc = kb.sb([8, 4])
        kb.dma('sp', scr, cin.rearrange("b (p kc) -> p b kc", kc=8), w=[kscr])
        kb.act(sc.rearrange("p kc b -> p b kc"), scr, AF.Silu, r=[kscr], w=[ksc])
        adab, kab = kb.sb([6 * D])
        adabT, kabT = kb.sb([48])
        modsb, kms = kb.sb([6 * D])
        aws = [kb.sb([8, 512]) for _ in range(2)]
        for l in sorted(set(l_ for (l_, _w) in self.cfg['steps'])):
            kb.dma('sp', adab[0:4], ada_b[l].partition_broadcast(4), w=[kab])
            kb.S.dma('sp', lambda e, l=l: e.dma_start(out=adabT, in_=ada_b[l].rearrange("(c p) -> p c", p=128),
                                                      allow_slow_non_contiguous=True), writes=[kabT])
            for nt in range(12):
                aw, kaw = aws[nt % 2]
                kb.dma('sp' if nt % 2 == 0 else 'pool', aw,
                       ada_w[l][:, nt * 512:(nt + 1) * 512].rearrange("(p kc) n -> p kc n", kc=8), w=[kaw])
                b0, kb0 = kb.bank(0)
                b1, kb1 = kb.bank(1)
                for kc in range(8):
                    kb.mm(b0[0:4, :], sc[:, kc, :], aw[:, kc, :], kc == 0, kc == 7, r=[ksc, kaw], w=[kb0])
                kb.tt('dve', modsb[0:4, nt * 512:(nt + 1) * 512], b0[0:4, :], adab[0:4, nt * 512:(nt + 1) * 512], ALU.add,
                      r=[kb0, kab], w=[kms])
                for q in range(4):
                    for kc in range(8):
                        kb.mm(b1[:, q * 4:(q + 1) * 4], aw[:, kc, q * 128:(q + 1) * 128], sc[:, kc, :], kc == 0, kc == 7,
                              r=[ksc, kaw], w=[kb1])
                b1v = b1[:, 0:16].rearrange("p (q b) -> p q b", q=4)
                for b in range(4):
                    kb.tt('dve', self.modT[:, l * 4 + b, nt * 4:(nt + 1) * 4], b1v[:, :, b], adabT[:, nt * 4:(nt + 1) * 4], ALU.add,
                          r=[kb1, kabT], w=[kmt])
            for j in (1, 4):
                v = self.modT[:, l * 4:(l + 1) * 4, j * 8:(j + 1) * 8]
                kb.ts('dve', v, v, 1.0, None, ALU.add, r=[kmt], w=[kmt])
            kb.dma('sp', self.modd[l], modsb[0:4, :], r=[kms], w=['modd'])

    def mod_col(self, l, bsel, j, kc):
        c = j * 8 + kc
        return self.modT[:, l * 4 + bsel, c:c + 1]

    def mt_tile(self, src, src_key, l, bsel, j0, hT, hT_key, xt, xt_key, bk):
        kb = self.kb
        kb.dma('sp', xt, src, r=[src_key], w=[xt_key])
        pv = [kb.bank(bk[0], [4, 128]), kb.bank(bk[1], [4, 128])]
        for kc in range(8):
            bv, bkey = pv[kc // 4]
            kb.tr(bv[:, kc % 4, :], xt[:, kc * 128:(kc + 1) * 128], self.identf, r=[xt_key], w=[bkey])
        for kc in range(8):
            bv, bkey = pv[kc // 4]
            sc_ = self.mod_col(l, bsel, j0 + 1, kc)
            sh_ = self.mod_col(l, bsel, j0, kc)
            if kc % 2 == 0:
                kb.act(hT[:, kc, :], bv[:, kc % 4, :], AF.Identity, bias=sh_, scale=sc_, r=[bkey, self.k_modT], w=[hT_key])
            else:
                kb.ts('dve', hT[:, kc, :], bv[:, kc % 4, :], sc_, sh_, ALU.mult, ALU.add, r=[bkey, self.k_modT], w=[hT_key])

    def ln_tile(self, u, ku, out, kout, lng, lnb, kln, st6, kst, mv, kmv):
        kb = self.kb
        kb.S.op('dve', lambda e: e.bn_stats(st6[:, 0, :], u[:, 0:512]), reads=[ku], writes=[kst])
        kb.S.op('dve', lambda e: e.bn_stats(st6[:, 1, :], u[:, 512:1024]), reads=[ku], writes=[kst])
        kb.S.op('dve', lambda e: e.bn_aggr(mv[:, 0:2], st6), reads=[kst], writes=[kmv])
        kb.ts('dve', mv[:, 2:3], mv[:, 1:2], EPS, None, ALU.add, r=[kmv], w=[kmv])
        kb.act(mv[:, 3:4], mv[:, 2:3], AF.Sqrt, r=[kmv], w=[kmv])
        kb.recip(mv[:, 4:5], mv[:, 3:4], r=[kmv], w=[kmv])
        kb.ts('dve', u, u, mv[:, 0:1], mv[:, 4:5], ALU.subtract, ALU.mult, r=[ku, kmv], w=[ku])
        kb.tt('pool', u, u, lng, ALU.mult, r=[ku, kln], w=[ku])
        kb.tt('dve', out, u, lnb, ALU.add, r=[ku, kln], w=[kout])

    @staticmethod
    def tile_of(gt):
        b, t = divmod(gt, NT)
        return b, t, (2 if t < 2 else b)

    def moe_phase(self, l, Xs, Xd, last):
        kb = self.kb
        p = self.W
        tiles = [gt for gt in range(2 * NT) if not (last and (gt % NT) < 2)]
        ngrp = 4
        per = (len(tiles) + ngrp - 1) // ngrp
        groups = [tiles[i * per:(i + 1) * per] for i in range(ngrp)]
        kb.phase()
        wr, kwr = kb.sb([8, 36], BF16)
        br, kbr = kb.sb([36])
        kb.dma('pool', wr, p['moe_wr'][l].rearrange("(kc p) n -> p kc n", p=128), w=[kwr])
        kb.dma('sp', br, p['moe_br'][l].partition_broadcast(128), w=[kbr])
        base_off = kb.off
        for grp in groups:
            kb.S.barrier()
            kb.off = base_off
            ng = len(grp)
            hT, khT = kb.sb([8, per * 128], BF16)
            G, kG = kb.sb([per, 32])
            acc, kacc = kb.sb([per, D])
            grp_off = kb.off
            xts = [kb.sb([D]) for _ in range(2)]
            rs = [kb.sb([64]) for _ in range(2)]
            for i, gt in enumerate(grp):
                b, t, bsel = self.tile_of(gt)
                xt, kxt = xts[i % 2]
                khi = khT + '_%d' % i
                self.mt_tile(Xs[b, t * 128:(t + 1) * 128, :], ('X', gt), l, bsel, 3, hT[:, :, i * 128:(i + 1) * 128], khi,
                             xt, kxt, (0, 1))
                pb, kpb = kb.bank(2 + i % 2)
                for kc in range(8):
                    kb.mm(pb[:, 0:36], hT[:, kc, i * 128:(i + 1) * 128], wr[:, kc, :], kc == 0, kc == 7, r=[khi, kwr], w=[kpb])
                R, kR = rs[i % 2]
                L = R[:, 0:36]
                kb.tt('dve', L, pb[:, 0:36], br, ALU.add, r=[kpb, kbr], w=[kR])
                gmax = R[:, 36:37]
                kb.red(gmax, L[:, 0:4], ALU.max, r=[kR], w=[kR])
                ngmax = R[:, 37:38]
                kb.ts('dve', ngmax, gmax, -1.0, None, ALU.mult, r=[kR], w=[kR])
                gsum = R[:, 38:39]
                kb.act(R[:, 40:44], L[:, 0:4], AF.Exp, bias=ngmax, accum=gsum, r=[kR], w=[kR])
                ggate = R[:, 39:40]
                kb.recip(ggate, gsum, r=[kR], w=[kR])
                ohg = R[:, 44:48]
                kb.ts('dve', ohg, L[:, 0:4], gmax, None, ALU.is_equal, r=[kR], w=[kR])
                ein = R[:, 48:56]
                kb.ts('dve', ein, L[:, 4:12], ohg[:, 0:1], None, ALU.mult, r=[kR], w=[kR])
                for g in range(1, 4):
                    kb.stt(ein, L[:, 4 + 8 * g:12 + 8 * g], ohg[:, g:g + 1], ein, ALU.mult, ALU.add, r=[kR], w=[kR])
                m1 = R[:, 56:57]
                kb.red(m1, ein, ALU.max, r=[kR], w=[kR])
                Grow = G[:, i, :]
                kGi = kG + '_%d' % i
                oh1 = Grow[:, 0:8]
                oh2 = Grow[:, 8:16]
                ein2 = Grow[:, 16:24]
                kb.ts('dve', oh1, ein, m1, None, ALU.is_equal, r=[kR], w=[kGi])
                kb.stt(ein2, oh1, -1.0e30, ein, ALU.mult, ALU.add, r=[kR, kGi], w=[kGi])
                m2 = R[:, 57:58]
                kb.red(m2, ein2, ALU.max, r=[kGi], w=[kR])
                kb.ts('dve', oh2, ein2, m2, None, ALU.is_equal, r=[kR, kGi], w=[kGi])
                dd = R[:, 58:59]
                kb.tt('dve', dd, m2, m1, ALU.subtract, r=[kR], w=[kR])
                ed = R[:, 59:60]
                kb.act(ed, dd, AF.Exp, r=[kR], w=[kR])
                den = R[:, 60:61]
                kb.ts('dve', den, ed, 1.0, None, ALU.add, r=[kR], w=[kR])
                w1 = R[:, 61:62]
                kb.recip(w1, den, r=[kR], w=[kR])
                g1 = R[:, 62:63]
                kb.tt('dve', g1, w1, ggate, ALU.mult, r=[kR], w=[kR])
                g2 = R[:, 63:64]
                kb.tt('dve', g2, g1, ed, ALU.mult, r=[kR], w=[kR])
                ge = Grow[:, 24:32]
                kb.ts('dve', ge, oh1, g1, None, ALU.mult, r=[kR, kGi], w=[kGi])
                kb.stt(ein, oh2, g2, ge, ALU.mult, ALU.add, r=[kR, kGi], w=[kR])
                for g in range(4):
                    kb.ts('dve', Grow[:, 8 * g:8 * g + 8], ein, ohg[:, g:g + 1], None, ALU.mult, r=[kR], w=[kGi])
            kb.S.barrier()
            kb.off = grp_off
            wb = [(kb.sb([8, 512], BF16), kb.sb([8, 512], BF16), kb.sb([4, D], BF16)) for _ in range(2)]
            sils = [kb.sb([512]) for _ in range(2)]
            aTs = [kb.sb([4, 512], BF16) for _ in range(2)]
            chunks = [(s, min(4, ng - s)) for s in range(0, ng, 4)]
            cc = 0
            oc = 0
            for e in range(32):
                (w1b, kw1), (w3b, kw3), (w2b, kw2) = wb[e % 2]
                kb.dma('pool', w1b, p['moe_w1'][l, e].rearrange("(kc p) n -> p kc n", p=128), w=[kw1])
                kb.dma('pool', w3b, p['moe_w3'][l, e].rearrange("(kc p) n -> p kc n", p=128), w=[kw3])
                kb.dma('pool', w2b, p['moe_w2'][l, e].rearrange("(kc p) n -> p kc n", p=128), w=[kw2])
                for (s, n) in chunks:
                    N = n * 128
                    aT, kaT = aTs[cc % 2]
                    cc += 1
                    hks = [khT + '_%d' % i for i in range(s, s + n)]
                    for hc in range(4):
                        A, kA = kb.bank((hc % 2) * 2)
                        B, kB = kb.bank((hc % 2) * 2 + 1)
                        for kc in range(8):
                            kb.mm(A[:, 0:N], w1b[:, kc, hc * 128:(hc + 1) * 128], hT[:, kc, s * 128:s * 128 + N], kc == 0, kc == 7,
                                  r=[kw1] + hks, w=[kA])
                        for kc in range(8):
                            kb.mm(B[:, 0:N], w3b[:, kc, hc * 128:(hc + 1) * 128], hT[:, kc, s * 128:s * 128 + N], kc == 0, kc == 7,
                                  r=[kw3] + hks, w=[kB])
                        sl, ksl = sils[hc % 2]
                        kb.act(sl[:, 0:N], A[:, 0:N], AF.Silu, r=[kA], w=[ksl])
                        kb.tt('dve', aT[:, hc, 0:N], sl[:, 0:N], B[:, 0:N], ALU.mult, r=[ksl, kB], w=[kaT + '_%d' % hc])
                    kas = [kaT + '_%d' % hc for hc in range(4)]
                    for ti in range(n):
                        i = s + ti
                        for nt in range(2):
                            O, kO = kb.bank(4 + oc % 4)
                            oc += 1
                            for hc in range(4):
                                kb.mm(O, aT[:, hc, ti * 128:(ti + 1) * 128], w2b[:, hc, nt * 512:(nt + 1) * 512], hc == 0, hc == 3,
                                      r=[kw2] + kas, w=[kO])
                            av = acc[:, i, nt * 512:(nt + 1) * 512]
                            ka = kacc + '_%d_%d' % (i, nt)
                            gcol = G[:, i, e:e + 1]
                            if e == 0:
                                kb.ts('dve', av, O, gcol, None, ALU.mult, r=[kO, kG + '_%d' % i], w=[ka])
                            else:
                                kb.stt(av, O, gcol, av, ALU.mult, ALU.add, r=[kO, kG + '_%d' % i, ka], w=[ka])
            kb.S.barrier()
            kb.off = grp_off
            lng, kln = kb.sb([D])
            lnb, _ = kb.sb([D])
            kb.dma('sp', lng, p['ln_g'][l, 1].partition_broadcast(128), w=[kln])
            kb.dma('sp', lnb, p['ln_b'][l, 1].partition_broadcast(128), w=[kln])
            gts = []
            for bsel in range(3):
                gt_, kgt = kb.sb([D])
                kb.dma('sp', gt_, self.modd[l, bsel, 5 * D:6 * D].partition_broadcast(128), r=['modd'], w=[kgt])
                gts.append((gt_, kgt))
            x1s = [kb.sb([D]) for _ in range(2)]
            outs = [kb.sb([D]) for _ in range(2)]
            sts = [kb.sb([2, 6]) for _ in range(2)]
            mvs = [kb.sb([8]) for _ in range(2)]
            for i, gt in enumerate(grp):
                b, t, bsel = self.tile_of(gt)
                x1, kx1 = x1s[i % 2]
                kb.dma('sp', x1, Xs[b, t * 128:(t + 1) * 128, :], r=[('X', gt)], w=[kx1])
                gtile, kgt = gts[bsel]
                av = acc[:, i, :]
                kas = [kacc + '_%d_%d' % (i, nt) for nt in range(2)]
                kb.tt('pool', av, av, gtile, ALU.mult, r=kas + [kgt], w=kas)
                kb.stt(x1, x1, ALPHA, av, ALU.mult, ALU.add, r=[kx1] + kas, w=[kx1])
                o, ko = outs[i % 2]
                st6, kst = sts[i % 2]
                mv, kmv = mvs[i % 2]
                self.ln_tile(x1, kx1, o, ko, lng, lnb, kln, st6, kst, mv, kmv)
                dst, kd = Xd(gt)
                kb.dma('sp', dst, o, r=[ko], w=[kd])


    def declare_weights(self):
        W = {}
        W['ln_g'] = self.inp('ln_g', [DEPTH, 2, D])
        W['ln_b'] = self.inp('ln_b', [DEPTH, 2, D])
        W['moe_wr'] = self.inp('moe_wr', [DEPTH, D, 36])
        W['moe_br'] = self.inp('moe_br', [DEPTH, 36])
        W['moe_w1'] = self.inp('moe_w1', [DEPTH, 32, D, 512])
        W['moe_w3'] = self.inp('moe_w3', [DEPTH, 32, D, 512])
        W['moe_w2'] = self.inp('moe_w2', [DEPTH, 32, 512, D])
        for nm, shp in (('o_w_in', [2, D, 1440]), ('o_w_out', [2, D, D]), ('o_mla_q_norm', [2, 384]), ('o_mla_kv_norm', [2, 256]),
                        ('o_w_uq', [2, 384, 768]), ('o_w_ukv', [2, 256, 1024]), ('o_gqa_q_norm', [2, 64]), ('o_gqa_k_norm', [2, 64]),
                        ('rope', [32, 128, 96])):
            W[nm] = self.inp(nm, shp)
        self.W = W
        sc = lambda nm, shp: [self.scratch('%s%d' % (nm, b), shp, BF16) for b in range(NB)]
        self.QTm = sc('QTm', [8, 97, LT])
        self.KTm = sc('KTm', [8, 97, LT])
        self.Vm = sc('Vm', [LT, 8, 65])
        self.QTg = sc('QTg', [8, 65, LT])
        self.KTg = sc('KTg', [2, 65, LT])
        self.Vg = sc('Vg', [LT, 2, 65])
        self.MIXT = sc('MIXT', [16, 64, LT])
        for nm, shp in (('e_w_in', [2, D, 4640]), ('e_w_out', [2, 1536, D]), ('e_conv_w', [2, 5, 2048]), ('e_conv_b', [2, 2048]),
                        ('e_dt_bias', [2, 2, 16]), ('e_a_log', [2, 2, 16]), ('e_d_skip', [2, 16]), ('e_gnorm_w', [2, D]),
                        ('nabias', [2, 8, 64, 45, 128]), ('tri', [2, 128, 128])):
            W[nm] = self.inp(nm, shp)
        self.QTn = sc('QTn', [8, 65, LT])
        self.KTn = sc('KTn', [8, 65, LT])
        self.Vn = sc('Vn', [LT, 8, 65])
        self.MIXE = sc('MIXE', [1536, LT])
        self.XBS = sc('XBS', [LT, 1536])
        self.BCT = sc('BCT', [1024, LT])
        self.ZS = sc('ZS', [LT, D])
        self.XBCT = [self.scratch('XBCT%d' % b, [2048, LT], F32) for b in range(NB)]
        self.DTA = [self.scratch('DTA%d' % b, [LT, 64], F32) for b in range(NB)]
        self.YD = [[self.scratch('YD%d_%d' % (d, b), [LT, D], F32) for b in range(NB)] for d in range(2)]

    def build(self):
        cfg = self.cfg
        self.declare_weights()
        self.xin = self.inp('xin', [NB, LT, D])
        dbg = cfg.get('debug_x', False)
        self.Y = self.scratch('y', [NB, LL, D], out=(not dbg))
        self.X = self.scratch('xres', [NB, LT, D], out=dbg)
        self.prologue()
        steps = cfg['steps']
        first = True
        for (l, what) in steps:
            src = self.xin if first else self.X
            first = False
            last = (l == DEPTH - 1)
            if what == 'mix':
                def Xd1(gt):
                    b, t = divmod(gt, NT)
                    return self.X[b, t * 128:(t + 1) * 128, :], ('X', gt)
                if l % 2 == 1:
                    self.odd_prep(l, src)
                    self.odd_attn(l)
                    self.odd_out(l, src, Xd1)
                else:
                    self.even_mixer(l, src, Xd1)
            if what == 'moe':
                if last and not dbg:
                    def Xd(gt):
                        b, t = divmod(gt, NT)
                        return self.Y[b, (t - 2) * 128:(t - 1) * 128, :], ('Y', gt)
                else:
                    def Xd(gt):
                        b, t = divmod(gt, NT)
                        return self.X[b, t * 128:(t + 1) * 128, :], ('X', gt)
                self.moe_phase(l, src, Xd, last)
        self.kb.S.emit()
        self.st.close()
        return self.nc


def host_inputs(inputs, core):
    f = lambda a: np.ascontiguousarray(np.asarray(a, dtype=np.float32))
    b0 = 2 * core
    m = {}
    m['cin'] = f(np.stack([inputs['c'][b0], inputs['c'][b0 + 1], inputs['c_ctx'], inputs['c_ctx']], 0))
    m['ident'] = np.eye(128, dtype=np.float32)
    return m


def shared_inputs(inputs):
    f = lambda a: np.ascontiguousarray(np.asarray(a, dtype=np.float32))
    m = {}
    for k in ('ada_w', 'ada_b', 'ln_g', 'ln_b', 'moe_w1', 'moe_w3', 'moe_w2', 'o_w_in', 'o_w_out', 'o_mla_q_norm', 'o_mla_kv_norm',
              'o_w_uq', 'o_w_ukv', 'o_gqa_q_norm', 'o_gqa_k_norm'):
        m[k] = f(inputs[k])
    m['rope'] = rope_tables()
    for k in ('e_w_in', 'e_w_out', 'e_conv_w', 'e_conv_b', 'e_dt_bias', 'e_a_log', 'e_d_skip', 'e_gnorm_w'):
        m[k] = f(inputs[k])
    m['nabias'] = np.stack([na_bias_tables(np.asarray(inputs['e_rpb'][i], np.float32)) for i in range(2)], 0)
    tri = np.triu(np.ones((128, 128), np.float32))
    m['tri'] = np.ascontiguousarray(np.stack([tri, tri.T], 0))
    m['moe_wr'] = f(np.concatenate([inputs['moe_wg'], inputs['moe_we']], axis=-1))
    m['moe_br'] = f(np.concatenate([inputs['moe_bg'], inputs['moe_be']], axis=-1))
    return m


MLA_SCALE = 96 ** -0.5
GQA_SCALE = 64 ** -0.5


def rope_tables():
    out = np.zeros((32, 128, 96), np.float32)
    pos = np.arange(LL)
    row, col = pos // 64, pos % 64
    for (half, o) in ((16, 0), (32, 32)):
        inv = np.power(np.float32(10000.0), -np.arange(0, half, 2, dtype=np.float32) / np.float32(half)).astype(np.float32)
        nf = half // 2
        ar = row.astype(np.float32)[:, None] * inv
        ac = col.astype(np.float32)[:, None] * inv
        cosv = np.concatenate([np.cos(ar), np.cos(ac)], 1).astype(np.float32)
        sinv = np.concatenate([np.sin(ar), np.sin(ac)], 1).astype(np.float32)
        out[:, :, o:o + 2 * nf] = cosv.reshape(32, 128, 2 * nf)
        out[:, :, o + 2 * nf:o + 4 * nf] = sinv.reshape(32, 128, 2 * nf)
    return out


def _odd_methods():
    def rope(self, dst, src, H, half, cos, sin, lat, kd, ks, ktab):
        kb = self.kb
        nf = half // 2
        if not lat:
            kb.cp('dve', dst, src, r=[ks], w=[kd])
            return
        t1, k1 = self.rt[0]
        t2, k2 = self.rt[1]
        for rc in range(2):
            v1 = src[:, :, rc * half:rc * half + nf]
            v2 = src[:, :, rc * half + nf:(rc + 1) * half]
            o1 = dst[:, :, rc * half:rc * half + nf]
            o2 = dst[:, :, rc * half + nf:(rc + 1) * half]
            c = cos[:, rc * nf:(rc + 1) * nf].unsqueeze(1).to_broadcast([128, H, nf])
            s = sin[:, rc * nf:(rc + 1) * nf].unsqueeze(1).to_broadcast([128, H, nf])
            a = t1[:, 0:H * nf].rearrange("p (h f) -> p h f", h=H)
            b_ = t2[:, 0:H * nf].rearrange("p (h f) -> p h f", h=H)
            kb.tt('dve', a, v1, c, ALU.mult, r=[ks, ktab], w=[k1])
            kb.tt('pool', b_, v2, s, ALU.mult, r=[ks, ktab], w=[k2])
            kb.tt('dve', o1, a, b_, ALU.subtract, r=[k1, k2], w=[kd])
            kb.tt('dve', a, v1, s, ALU.mult, r=[ks, ktab], w=[k1])
            kb.tt('pool', b_, v2, c, ALU.mult, r=[ks, ktab], w=[k2])
            kb.tt('dve', o2, a, b_, ALU.add, r=[k1, k2], w=[kd])

    def rstd_from_ssq(self, out, ssq, n, k):
        kb = self.kb
        kb.ts('dve', out, ssq, 1.0 / n, EPS, ALU.mult, ALU.add, r=[k], w=[k])
        kb.act(out, out, AF.Sqrt, r=[k], w=[k])
        kb.recip(out, out, r=[k], w=[k])

    def odd_prep(self, l, Xs):
        kb = self.kb
        p = self.W
        i = l // 2
        kb.phase()
        bf = lambda shape: kb.sb(shape, BF16)
        win, kwin = bf([8, 1440])
        wuq, kwuq = bf([3, 768])
        wukv, kwukv = bf([2, 1024])
        kb.dma('pool', win, p['o_w_in'][i].rearrange("(kc p) n -> p kc n", p=128), w=[kwin])
        kb.dma('pool', wuq, p['o_w_uq'][i].rearrange("(kc p) n -> p kc n", p=128), w=[kwuq])
        kb.dma('pool', wukv, p['o_w_ukv'][i].rearrange("(kc p) n -> p kc n", p=128), w=[kwukv])
        gq, kgq = kb.sb([384])
        gkv, kgkv = kb.sb([256])
        ggq, kggq = kb.sb([64])
        ggk, kggk = kb.sb([64])
        kb.dma('sp', gq, p['o_mla_q_norm'][i].partition_broadcast(128), w=[kgq])
        kb.dma('sp', gkv, p['o_mla_kv_norm'][i].partition_broadcast(128), w=[kgkv])
        kb.dma('sp', ggq, p['o_gqa_q_norm'][i].partition_broadcast(128), w=[kggq])
        kb.dma('sp', ggk, p['o_gqa_k_norm'][i].partition_broadcast(128), w=[kggk])
        kb.ts('dve', ggq, ggq, GQA_SCALE, None, ALU.mult, r=[kggq], w=[kggq])
        ropet, krope = kb.sb([32, 96])
        kb.dma('sp', ropet, p['rope'].rearrange("t p c -> p t c"), w=[krope])
        kmr, kkmr = kb.sb([16])
        ones, kones = bf([LT])
        kb.memset('pool', ones, 1.0, w=[kones])
        xts = [kb.sb([D]) for _ in range(2)]
        hTs = [bf([8, 128]) for _ in range(2)]
        pj, kpj = kb.sb([1440])
        sm, ksm = kb.sb([64])
        cqn, kcqn = bf([384])
        cqnT, kcqnT = bf([3, 128])
        ckvn, kckvn = bf([256])
        ckvnT, kckvnT = bf([2, 128])
        q, kq = kb.sb([8, 96])
        kv, kkv = kb.sb([8, 128])
        tmp, ktmp = kb.sb([1024])
        gn, kgn = kb.sb([10, 64])
        self.rt = [kb.sb([256]) for _ in range(2)]
        qa, kqa = bf([8, 98])
        ka, kka = bf([8, 98])
        va, kva = bf([8, 66])
        qga, kqga = bf([8, 66])
        kga, kkga = bf([2, 66])
        vga, kvga = bf([2, 66])
        stq, kstq = bf([8, 128])
        stk, kstk = bf([8, 128])
        stg, kstg = bf([8, 128])
        stkg, kstkg = bf([2, 128])
        krw, kkrw = bf([LT])
        for b in range(NB):
            kb.memset('dve', kmr, 0.0, w=[kkmr])
            for t in range(NT):
                lat = t >= 2
                bsel = b if lat else 2
                gt = b * NT + t
                xt, kxt = xts[t % 2]
                hT, khT = hTs[t % 2]
                self.mt_tile(Xs[b, t * 128:(t + 1) * 128, :], ('X', gt), l, bsel, 0, hT, khT, xt, kxt, (0, 1))
                for nt, (c0, c1) in enumerate(((0, 512), (512, 1024), (1024, 1440))):
                    pb, kpb = kb.bank(2 + nt)
                    for kc in range(8):
                        kb.mm(pb[:, 0:c1 - c0], hT[:, kc, :], win[:, kc, c0:c1], kc == 0, kc == 7, r=[khT, kwin], w=[kpb])
                    kb.cp('act', pj[:, c0:c1], pb[:, 0:c1 - c0], r=[kpb], w=[kpj])
                if lat:
                    tb = ropet[:, t - 2, :]
                    cm, sm_, cg, sg = tb[:, 0:16], tb[:, 16:32], tb[:, 32:64], tb[:, 64:96]
                else:
                    cm = sm_ = cg = sg = None
                kb.act(tmp[:, 0:384], pj[:, 0:384], AF.Square, accum=sm[:, 0:1], r=[kpj], w=[ktmp, ksm])
                self.rstd_from_ssq(sm[:, 0:1], sm[:, 0:1], 384, ksm)
                kb.stt(cqn, pj[:, 0:384], sm[:, 0:1], gq, ALU.mult, ALU.mult, r=[kpj, ksm, kgq], w=[kcqn])
                pT, kpT = kb.bank(5, [8, 128], BF16)
                for c in range(3):
                    kb.tr(pT[:, c, :], cqn[:, c * 128:(c + 1) * 128], self.identb, r=[kcqn], w=[kpT])
                kb.cp('dve', cqnT, pT[:, 0:3, :], r=[kpT], w=[kcqnT])
                qf = q.rearrange("p h d -> p (h d)")
                for nt, (c0, c1) in enumerate(((0, 512), (512, 768))):
                    pb, kpb = kb.bank(2 + nt)
                    for c in range(3):
                        kb.mm(pb[:, 0:c1 - c0], cqnT[:, c, :], wuq[:, c, c0:c1], c == 0, c == 2, r=[kcqnT, kwuq], w=[kpb])
                    kb.act(qf[:, c0:c1], pb[:, 0:c1 - c0], AF.Copy, scale=MLA_SCALE, r=[kpb], w=[kq])
                kb.cp('dve', qa[:, :, 0:64], q[:, :, 0:64], r=[kq], w=[kqa])
                self.rope(qa[:, :, 64:96], q[:, :, 64:96], 8, 16, cm, sm_, lat, kqa, kq, krope)
                t3 = tmp[:, 0:768].rearrange("p (h d) -> p h d", h=8)
                kb.tt('pool', t3, q, q, ALU.mult, r=[kq], w=[ktmp])
                kb.red(sm[:, 8:16], t3, ALU.add, r=[ktmp], w=[ksm])
                kb.act(sm[:, 8:16], sm[:, 8:16], AF.Sqrt, r=[ksm], w=[ksm])
                kb.ts('dve', qa[:, :, 96], sm[:, 8:16], -1.0, None, ALU.mult, r=[ksm], w=[kqa])
                kb.act(tmp[:, 0:256], pj[:, 896:1152], AF.Square, accum=sm[:, 1:2], r=[kpj], w=[ktmp, ksm])
                self.rstd_from_ssq(sm[:, 1:2], sm[:, 1:2], 256, ksm)
                kb.stt(ckvn, pj[:, 896:1152], sm[:, 1:2], gkv, ALU.mult, ALU.mult, r=[kpj, ksm, kgkv], w=[kckvn])
                for c in range(2):
                    kb.tr(pT[:, 4 + c, :], ckvn[:, c * 128:(c + 1) * 128], self.identb, r=[kckvn], w=[kpT])
                kb.cp('dve', ckvnT, pT[:, 4:6, :], r=[kpT], w=[kckvnT])
                kvf = kv.rearrange("p h d -> p (h d)")
                for nt in range(2):
                    pb, kpb = kb.bank(2 + nt)
                    for c in range(2):
                        kb.mm(pb, ckvnT[:, c, :], wukv[:, c, nt * 512:(nt + 1) * 512], c == 0, c == 1, r=[kckvnT, kwukv], w=[kpb])
                    kb.cp('act', kvf[:, nt * 512:(nt + 1) * 512], pb, r=[kpb], w=[kkv])
                kb.cp('dve', ka[:, :, 0:64], kv[:, :, 0:64], r=[kkv], w=[kka])
                kb.cp('pool', va[:, :, 0:64], kv[:, :, 64:128], r=[kkv], w=[kva])
                kb.memset('pool', va[:, :, 64:65], 1.0, w=[kva])
                kr = gn[:, 9, 0:32]
                self.rope(kr.unsqueeze(1), pj[:, 1152:1184].unsqueeze(1), 1, 16, cm, sm_, lat, kgn, kpj, krope)
                kb.cp('dve', ka[:, :, 64:96], kr.unsqueeze(1).to_broadcast([128, 8, 32]), r=[kgn], w=[kka])
                t4 = tmp[:, 0:512].rearrange("p (h d) -> p h d", h=8)
                kb.tt('pool', t4, kv[:, :, 0:64], kv[:, :, 0:64], ALU.mult, r=[kkv], w=[ktmp])
                kb.red(sm[:, 16:24], t4, ALU.add, r=[ktmp], w=[ksm])
                kb.act(tmp[:, 512:544], pj[:, 1152:1184], AF.Square, accum=sm[:, 2:3], r=[kpj], w=[ktmp, ksm])
                kb.ts('dve', sm[:, 16:24], sm[:, 16:24], sm[:, 2:3], None, ALU.add, r=[ksm], w=[ksm])
                kb.tt('dve', kmr[:, 0:8], kmr[:, 0:8], sm[:, 16:24], ALU.max, r=[ksm, kkmr], w=[kkmr])
                for (src, H, gtab, kg, dst, kdst, s0) in ((pj[:, 384:896], 8, ggq, kggq, qga, kqga, 24),
                                                          (pj[:, 1184:1312], 2, ggk, kggk, kga, kkga, 40)):
                    sv = src.rearrange("p (h d) -> p h d", h=H)
                    tv = tmp[:, 0:H * 64].rearrange("p (h d) -> p h d", h=H)
                    kb.tt('pool', tv, sv, sv, ALU.mult, r=[kpj], w=[ktmp])
                    ss = sm[:, s0:s0 + H]
                    kb.red(ss, tv, ALU.add, r=[ktmp], w=[ksm])
                    self.rstd_from_ssq(ss, ss, 64, ksm)
                    gv_ = gn[:, 0:H, :]
                    kb.tt('dve', gv_, sv, ss.unsqueeze(2).to_broadcast([128, H, 64]), ALU.mult, r=[kpj, ksm], w=[kgn])
                    kb.tt('dve', gv_, gv_, gtab.unsqueeze(1).to_broadcast([128, H, 64]), ALU.mult, r=[kgn, kg], w=[kgn])
                    self.rope(dst[:, :, 0:64], gv_, H, 32, cg, sg, lat, kdst, kgn, krope)
                    kb.tt('pool', tv, gv_, gv_, ALU.mult, r=[kgn], w=[ktmp])
                    ss2 = sm[:, s0 + 8:s0 + 8 + H]
                    kb.red(ss2, tv, ALU.add, r=[ktmp], w=[ksm])
                    if H == 8:
                        kb.act(ss2, ss2, AF.Sqrt, r=[ksm], w=[ksm])
                        kb.ts('dve', dst[:, :, 64], ss2, -1.0, None, ALU.mult, r=[ksm], w=[kdst])
                    else:
                        kb.tt('dve', kmr[:, 8:10], kmr[:, 8:10], ss2, ALU.max, r=[ksm, kkmr], w=[kkmr])
                gvv = pj[:, 1312:1440].rearrange("p (h d) -> p h d", h=2)
                kb.cp('dve', vga[:, :, 0:64], gvv, r=[kpj], w=[kvga])
                kb.memset('pool', vga[:, :, 64:65], 1.0, w=[kvga])
                tok = slice(t * 128, (t + 1) * 128)
                for (srcb, ksrc, H, dd, stg_, kst_, dst_d, bkn) in (
                        (qa, kqa, 8, 97, stq, kstq, self.QTm[b], 6), (ka, kka, 8, 96, stk, kstk, self.KTm[b], 7),
                        (qga, kqga, 8, 65, stg, kstg, self.QTg[b], 6), (kga, kkga, 2, 64, stkg, kstkg, self.KTg[b], 7)):
                    pT2, kpT2 = kb.bank(bkn, [8, 128], BF16)
                    for h in range(H):
                        kb.tr(pT2[0:dd, h, :], srcb[:, h, 0:dd], self.identb, r=[ksrc], w=[kpT2])
                    kb.cp('act', stg_[0:dd, 0:H, :], pT2[0:dd, 0:H, :], r=[kpT2], w=[kst_])
                    kb.dma('sp', dst_d[:, 0:dd, tok].rearrange("h d t -> d h t"), stg_[0:dd, 0:H, :], r=[kst_])
                kb.dma('sp', self.Vm[b][tok, :, :], va[:, :, 0:65], r=[kva])
                kb.dma('sp', self.Vg[b][tok, :, :], vga[:, :, 0:65], r=[kvga])
            pk, kpk = kb.bank(5)
            kb.tr(pk[0:10, 0:128], kmr[:, 0:10], self.identf, r=[kkmr], w=[kpk])
            kb.red(sm[0:10, 60:61], pk[0:10, 0:128], ALU.max, r=[kpk], w=[ksm])
            kb.act(sm[0:10, 60:61], sm[0:10, 60:61], AF.Sqrt, r=[ksm], w=[ksm])
            kb.ts('dve', krw[0:10, :], ones[0:10, :], sm[0:10, 60:61], None, ALU.mult, r=[ksm, kones], w=[kkrw])
            for h in range(8):
                kb.dma('sp', self.KTm[b][h, 96:97, :], krw[h:h + 1, :], r=[kkrw])
            for h in range(2):
                kb.dma('sp', self.KTg[b][h, 64:65, :], krw[8 + h:9 + h, :], r=[kkrw])

    return dict(rope=rope, rstd_from_ssq=rstd_from_ssq, odd_prep=odd_prep)


for _k, _v in _odd_methods().items():
    setattr(Prog, _k, _v)


def _attn_methods():
    def attn_core(self, jobs, bias_fn=None):
        kb = self.kb
        bf = lambda shape: kb.sb(shape, BF16)
        KTs = [bf([LT]) for _ in range(2)]
        Vs = [bf([NT, 66]) for _ in range(2)]
        QTs = [bf([512]) for _ in range(2)]
        PTs = [bf([512]) for _ in range(3)]
        osbs = [kb.sb([512]) for _ in range(2)]
        rinv, krinv = kb.sb([512])
        onesf, konesf = kb.sb([64])
        mixs = [bf([512]) for _ in range(2)]
        kb.memset('dve', onesf, 1.0, w=[konesf])
        kb.memset('dve', rinv, 0.0, w=[krinv])
        qc = 0
        pc = 0
        lc = 0
        lastKV = None
        for ji, job in enumerate(jobs):
            dk, nkt = job['dk'], job['nkt']
            if job.get('load', True):
                KT, kKT = KTs[lc % 2]
                V, kV = Vs[lc % 2]
                lc += 1
                kb.dma('sp', KT[0:dk, 0:nkt * 128], job['KT'][:, 0:nkt * 128], w=[kKT])
                kb.S.dma('pool', lambda e, V=V, job=job, nkt=nkt: e.dma_start(
                    out=V[:, 0:nkt, 0:65], in_=job['V'][0:nkt * 128, :].rearrange("(t p) c -> p t c", p=128)), writes=[kV])
                lastKV = (KT, kKT, V, kV)
            else:
                KT, kKT, V, kV = lastKV
            for (QTd, N, nk, outd) in job['qblocks']:
                QT, kQT = QTs[qc % 2]
                O, kO = kb.bank(4 + qc % 2)
                osb, kosb = osbs[qc % 2]
                mix, kmix = mixs[qc % 2]
                qc += 1
                kb.dma('sp', QT[0:dk, 0:N], QTd, w=[kQT])
                for kt in range(nk):
                    S_, kS = kb.bank(pc % 3)
                    PT, kPT = PTs[pc % 3]
                    pc += 1
                    kb.mm(S_[:, 0:N], KT[0:dk, kt * 128:(kt + 1) * 128], QT[0:dk, 0:N], True, True, r=[kKT, kQT], w=[kS])
                    kb.act(PT[:, 0:N], S_[:, 0:N], AF.Exp, r=[kS], w=[kPT])
                    kb.mm(O[0:65, 0:N], V[:, kt, 0:65], PT[:, 0:N], kt == 0, kt == nk - 1, r=[kV, kPT], w=[kO])
                kb.cp('act', osb[0:65, 0:N], O[0:65, 0:N], r=[kO], w=[kosb])
                kb.recip(rinv[64:65, 0:N], osb[64:65, 0:N], r=[kosb], w=[krinv])
                Bc, kBc = kb.bank(6 + qc % 2)
                kb.mm(Bc[0:64, 0:N], onesf[0:65, 0:64], rinv[0:65, 0:N], True, True, r=[konesf, krinv], w=[kBc])
                kb.tt('dve', mix[0:64, 0:N], osb[0:64, 0:N], Bc[0:64, 0:N], ALU.mult, r=[kosb, kBc], w=[kmix])
                kb.dma('sp', outd, mix[0:64, 0:N], r=[kmix])

    def odd_attn(self, l):
        kb = self.kb
        kb.phase()
        ctxq = l < DEPTH - 1
        jobs = []
        for b in range(NB):
            for h in range(16):
                if h < 8:
                    KT, V, QT, dk = self.KTm[b][h], self.Vm[b][:, h, :], self.QTm[b][h], 97
                    load = True
                else:
                    g = (h - 8) // 4
                    KT, V, QT, dk = self.KTg[b][g], self.Vg[b][:, g, :], self.QTg[b][h - 8], 65
                    load = ((h - 8) % 4 == 0)
                qbs = []
                if ctxq:
                    qbs.append((QT[:, 0:256], 256, 2, self.MIXT[b][h, :, 0:256]))
                for qb in range(8):
                    s = 256 + qb * 512
                    qbs.append((QT[:, s:s + 512], 512, NT, self.MIXT[b][h, :, s:s + 512]))
                jobs.append(dict(KT=KT, V=V, dk=dk, nkt=NT, qblocks=qbs, load=load))
        self.attn_core(jobs)

    def mix_out(self, l, Xs, Xd, wout_ap, nk, kparts, mix_loader, last_ctx_skip):
        kb = self.kb
        p = self.W
        kb.phase()
        wo, kwo = kb.sb([nk, D], BF16)
        kb.dma('pool', wo[0:kparts], wout_ap, w=[kwo])
        lng, kln = kb.sb([D])
        lnb, _ = kb.sb([D])
        kb.dma('sp', lng, p['ln_g'][l, 0].partition_broadcast(128), w=[kln])
        kb.dma('sp', lnb, p['ln_b'][l, 0].partition_broadcast(128), w=[kln])
        gts = []
        for bsel in range(3):
            gt_, kgt = kb.sb([D])
            kb.dma('sp', gt_, self.modd[l, bsel, 2 * D:3 * D].partition_broadcast(128), r=['modd'], w=[kgt])
            gts.append((gt_, kgt))
        mts = [kb.sb([nk, 128], BF16) for _ in range(2)]
        x1s = [kb.sb([D]) for _ in range(2)]
        ys = [kb.sb([D]) for _ in range(2)]
        outs = [kb.sb([D]) for _ in range(2)]
        sts = [kb.sb([2, 6]) for _ in range(2)]
        mvs = [kb.sb([8]) for _ in range(2)]
        ii = 0
        for gt in range(2 * NT):
            b, t, bsel = self.tile_of(gt)
            if last_ctx_skip and t < 2:
                continue
            mt, kmt_ = mts[ii % 2]
            x1, kx1 = x1s[ii % 2]
            y, ky = ys[ii % 2]
            o, ko = outs[ii % 2]
            st6, kst = sts[ii % 2]
            mv, kmv = mvs[ii % 2]
            mix_loader(gt, mt, kmt_)
            kb.dma('sp', x1, Xs[b, t * 128:(t + 1) * 128, :], r=[('X', gt)], w=[kx1])
            gtile, kgt = gts[bsel]
            for nt in range(2):
                Y, kY = kb.bank(2 * (ii % 2) + nt)
                for c in range(nk):
                    kb.mm(Y, mt[0:kparts, c, :], wo[0:kparts, c, nt * 512:(nt + 1) * 512], c == 0, c == nk - 1, r=[kmt_, kwo], w=[kY])
                kb.tt('dve', y[:, nt * 512:(nt + 1) * 512], Y, gtile[:, nt * 512:(nt + 1) * 512], ALU.mult, r=[kY, kgt], w=[ky])
            kb.stt(x1, x1, ALPHA, y, ALU.mult, ALU.add, r=[kx1, ky], w=[kx1])
            self.ln_tile(x1, kx1, o, ko, lng, lnb, kln, st6, kst, mv, kmv)
            dst, kd = Xd(gt)
            kb.dma('sp', dst, o, r=[ko], w=[kd])
            ii += 1

    def odd_out(self, l, Xs, Xd):
        kb = self.kb
        i = l // 2

        def loader(gt, mt, kmt_):
            b, t = divmod(gt, NT)
            kb.dma('sp', mt[0:64], self.MIXT[b][:, :, t * 128:(t + 1) * 128].rearrange("h d t -> d h t"), w=[kmt_])
        self.mix_out(l, Xs, Xd, self.W['o_w_out'][i].rearrange("(h d) n -> d h n", d=64), 16, 64, loader, l == DEPTH - 1)

    return dict(attn_core=attn_core, odd_attn=odd_attn, mix_out=mix_out, odd_out=odd_out)


for _k, _v in _attn_methods().items():
    setattr(Prog, _k, _v)


NA_SCALE = 64 ** -0.5


def na_bias_tables(rpb):
    out = np.full((8, 5, 9, 64, 128), -30000.0, np.float32)
    for cls, m in enumerate((2, 0, 1, 30, 31)):
        R0 = min(max(2 * m - 4, 0), 55)
        for qi in range(128):
            r, j = 2 * m + qi // 64, qi % 64
            r0 = min(max(r - 4, 0), 56)
            c0 = min(max(j - 8, 0), 48)
            for kr in range(r0, r0 + 8):
                seg = kr - R0
                cols = np.arange(c0, c0 + 16)
                out[:, cls, seg, cols, qi] = rpb[:, kr - r + 7, cols - j + 15]
    return np.ascontiguousarray(out.transpose(0, 3, 1, 2, 4)).reshape(8, 64, 45, 128)


def _even_methods():
    def even_mixer(self, l, Xs, Xd):
        kb = self.kb
        p = self.W
        i = l // 2
        bf = lambda shape: kb.sb(shape, BF16)
        ctxq = True
        kb.phase()
        win, kwin = bf([8, 4640])
        kb.dma('pool', win, p['e_w_in'][i].rearrange("(kc p) n -> p kc n", p=128), w=[kwin])
        dtb, kdtb = kb.sb([32])
        kb.dma('sp', dtb, p['e_dt_bias'][i].rearrange("a b -> (a b)").partition_broadcast(128), w=[kdtb])
        Aneg, kA = kb.sb([32])
        kb.dma('sp', Aneg, p['e_a_log'][i].rearrange("a b -> (a b)").partition_broadcast(128), w=[kA])
        kb.act(Aneg, Aneg, AF.Exp, r=[kA], w=[kA])
        kb.ts('dve', Aneg, Aneg, -1.0, None, ALU.mult, r=[kA], w=[kA])
        kmr, kkmr = kb.sb([8])
        ones, kones = bf([LT])
        kb.memset('pool', ones, 1.0, w=[kones])
        krw, kkrw = bf([LT])
        xts = [kb.sb([D]) for _ in range(2)]
        hT4, khT4 = bf([8, 512])
        ev, kev = kb.sb([1536])
        sm, ksm = kb.sb([64])
        tmp, ktmp = kb.sb([512])
        qa, kqa = bf([8, 66])
        ka, kka = bf([8, 66])
        va, kva = bf([8, 66])
        zs, kzs = bf([D])
        dts, kdts = kb.sb([64])
        stq, kstq = bf([8, 128])
        stk, kstk = bf([8, 128])
        xo, kxo = kb.sb([512])
        for b in range(NB):
            kb.memset('dve', kmr, 0.0, w=[kkmr])
            for (t0, n) in [(0, 2)] + [(2 + 4 * s, 4) for s in range(8)]:
                N = n * 128
                for ti in range(n):
                    t = t0 + ti
                    bsel = b if t >= 2 else 2
                    xt, kxt = xts[ti % 2]
                    self.mt_tile(Xs[b, t * 128:(t + 1) * 128, :], ('X', b * NT + t), l, bsel, 0, hT4[:, :, ti * 128:(ti + 1) * 128],
                                 khT4 + '_%d' % ti, xt, kxt, (0, 1))
                hks = [khT4 + '_%d' % ti for ti in range(n)]
                for c in range(16):
                    pb, kpb = kb.bank(2 + c % 2)
                    for kc in range(8):
                        kb.mm(pb[:, 0:N], win[:, kc, 2560 + c * 128:2560 + (c + 1) * 128], hT4[:, kc, 0:N], kc == 0, kc == 7,
                              r=[kwin] + hks, w=[kpb])
                    kb.cp('act' if c % 2 == 0 else 'dve', xo[:, 0:N], pb[:, 0:N], r=[kpb], w=[kxo])
                    kb.dma('sp', self.XBCT[b][c * 128:(c + 1) * 128, t0 * 128:t0 * 128 + N], xo[:, 0:N], r=[kxo])
                for ti in range(n):
                    t = t0 + ti
                    tok = slice(t * 128, (t + 1) * 128)
                    hk = [khT4 + '_%d' % ti]
                    hTt = hT4[:, :, ti * 128:(ti + 1) * 128]
                    for nt in range(3):
                        pb, kpb = kb.bank(4 + nt)
                        for kc in range(8):
                            kb.mm(pb, hTt[:, kc, :], win[:, kc, nt * 512:(nt + 1) * 512], kc == 0, kc == 7, r=[kwin] + hk, w=[kpb])
                        if nt == 0:
                            kb.act(ev[:, 0:512], pb, AF.Copy, scale=NA_SCALE, r=[kpb], w=[kev])
                        else:
                            kb.cp('act', ev[:, nt * 512:(nt + 1) * 512], pb, r=[kpb], w=[kev])
                    for nt in range(2):
                        pb, kpb = kb.bank(4 + nt)
                        for kc in range(8):
                            kb.mm(pb, hTt[:, kc, :], win[:, kc, 1536 + nt * 512:1536 + (nt + 1) * 512], kc == 0, kc == 7, r=[kwin] + hk, w=[kpb])
                        kb.act(zs[:, nt * 512:(nt + 1) * 512], pb, AF.Silu, r=[kpb], w=[kzs])
                    kb.dma('sp', self.ZS[b][tok, :], zs, r=[kzs])
                    pb, kpb = kb.bank(6)
                    for kc in range(8):
                        kb.mm(pb[:, 0:32], hTt[:, kc, :], win[:, kc, 4608:4640], kc == 0, kc == 7, r=[kwin] + hk, w=[kpb])
                    xr = dts[:, 0:32]
                    kb.tt('dve', xr, pb[:, 0:32], dtb, ALU.add, r=[kpb, kdtb], w=[kdts])
                    ab = sm[:, 0:32]
                    kb.stt(ab, xr, -1.0, xr, ALU.mult, ALU.max, r=[kdts], w=[ksm])
                    kb.act(ab, ab, AF.Exp, scale=-1.0, r=[ksm], w=[ksm])
                    kb.act(ab, ab, AF.Ln, bias=1.0, r=[ksm], w=[ksm])
                    kb.stt(xr, xr, 0.0, ab, ALU.max, ALU.add, r=[kdts, ksm], w=[kdts])
                    kb.tt('dve', dts[:, 32:64], xr, Aneg, ALU.mult, r=[kdts, kA], w=[kdts])
                    kb.dma('sp', self.DTA[b][tok, :], dts, r=[kdts])
                    q3 = ev[:, 0:512].rearrange("p (h d) -> p h d", h=8)
                    k3 = ev[:, 512:1024].rearrange("p (h d) -> p h d", h=8)
                    v3 = ev[:, 1024:1536].rearrange("p (h d) -> p h d", h=8)
                    t3 = tmp.rearrange("p (h d) -> p h d", h=8)
                    kb.cp('dve', qa[:, :, 0:64], q3, r=[kev], w=[kqa])
                    kb.tt('pool', t3, q3, q3, ALU.mult, r=[kev], w=[ktmp])
                    kb.red(sm[:, 32:40], t3, ALU.add, r=[ktmp], w=[ksm])
                    kb.act(sm[:, 32:40], sm[:, 32:40], AF.Sqrt, r=[ksm], w=[ksm])
                    kb.ts('dve', qa[:, :, 64], sm[:, 32:40], -1.0, None, ALU.mult, r=[ksm], w=[kqa])
                    kb.cp('dve', ka[:, :, 0:64], k3, r=[kev], w=[kka])
                    kb.tt('pool', t3, k3, k3, ALU.mult, r=[kev], w=[ktmp])
                    kb.red(sm[:, 40:48], t3, ALU.add, r=[ktmp], w=[ksm])
                    kb.tt('dve', kmr, kmr, sm[:, 40:48], ALU.max, r=[ksm, kkmr], w=[kkmr])
                    kb.cp('pool', va[:, :, 0:64], v3, r=[kev], w=[kva])
                    kb.memset('pool', va[:, :, 64:65], 1.0, w=[kva])
                    for (srcb, ksrc, dd, stg_, kst_, dst_d, bkn) in ((qa, kqa, 65, stq, kstq, self.QTn[b], 6), (ka, kka, 64, stk, kstk, self.KTn[b], 7)):
                        pT2, kpT2 = kb.bank(bkn, [8, 128], BF16)
                        for h in range(8):
                            kb.tr(pT2[0:dd, h, :], srcb[:, h, 0:dd], self.identb, r=[ksrc], w=[kpT2])
                        kb.cp('act', stg_[0:dd, :, :], pT2[0:dd, :, :], r=[kpT2], w=[kst_])
                        kb.dma('sp', dst_d[:, 0:dd, tok].rearrange("h d t -> d h t"), stg_[0:dd, :, :], r=[kst_])
                    kb.dma('sp', self.Vn[b][tok, :, :], va[:, :, 0:65], r=[kva])
            pk, kpk = kb.bank(5)
            kb.tr(pk[0:8, 0:128], kmr[:, 0:8], self.identf, r=[kkmr], w=[kpk])
            kb.red(sm[0:8, 60:61], pk[0:8, 0:128], ALU.max, r=[kpk], w=[ksm])
            kb.act(sm[0:8, 60:61], sm[0:8, 60:61], AF.Sqrt, r=[ksm], w=[ksm])
            kb.ts('dve', krw[0:8, :], ones[0:8, :], sm[0:8, 60:61], None, ALU.mult, r=[ksm, kones], w=[kkrw])
            for h in range(8):
                kb.dma('sp', self.KTn[b][h, 64:65, :], krw[h:h + 1, :], r=[kkrw])
        self.na_attn(l)
        self.ssd_conv(l)
        self.ssd_scan(l)
        self.ssd_gate(l)

        def loader(gt, mt, kmt_):
            b, t = divmod(gt, NT)
            kb.dma('sp', mt, self.MIXE[b][:, t * 128:(t + 1) * 128].rearrange("(c p) t -> p c t", p=128), w=[kmt_])
        self.mix_out(l, Xs, Xd, p['e_w_out'][i].rearrange("(c p) n -> p c n", p=128), 12, 128, loader, False)

    def na_attn(self, l):
        kb = self.kb
        p = self.W
        i = l // 2
        kb.phase()
        bf = lambda shape: kb.sb(shape, BF16)
        KTs = [bf([LT]) for _ in range(2)]
        Vs = [bf([68, 66]) for _ in range(2)]
        BTs = [bf([5 * 9, 128]) for _ in range(2)]
        QTs = [bf([128]) for _ in range(2)]
        PTs = [bf([512]) for _ in range(3)]
        osbs = [kb.sb([128]) for _ in range(2)]
        rinv, krinv = kb.sb([128])
        onesf, konesf = kb.sb([64])
        mixs = [bf([128]) for _ in range(2)]
        kb.memset('dve', onesf, 1.0, w=[konesf])
        kb.memset('dve', rinv, 0.0, w=[krinv])
        qc = 0
        pc = 0
        for b in range(NB):
            for h in range(8):
                j = b * 8 + h
                KT, kKT = KTs[j % 2]
                V, kV = Vs[j % 2]
                BT, kBT = BTs[j % 2]
                kb.dma('sp', KT[0:65, :], self.KTn[b][h], w=[kKT])
                kb.dma('pool', V[0:64, :, 0:65], self.Vn[b][:, h, :].rearrange("(t p) c -> p t c", p=64), w=[kV])
                kb.dma('pool', BT[0:64], p['nabias'][i, h], w=[kBT])
                for qt in range(NT):
                    if qt < 2:
                        segs = [(s * 64, None) for s in range(4)]
                    else:
                        m = qt - 2
                        cls = {0: 1, 1: 2, 30: 3, 31: 4}.get(m, 0)
                        R0 = min(max(2 * m - 4, 0), 55)
                        segs = [(s * 64, None) for s in range(4)] + [(256 + (R0 + s) * 64, cls * 9 + s) for s in range(9)]
                    QT, kQT = QTs[qc % 2]
                    O, kO = kb.bank(4 + qc % 2)
                    osb, kosb = osbs[qc % 2]
                    mix, kmix = mixs[qc % 2]
                    qc += 1
                    tok = slice(qt * 128, (qt + 1) * 128)
                    kb.dma('sp', QT[0:65, :], self.QTn[b][h][:, tok], w=[kQT])
                    ns = len(segs)
                    for g0 in range(0, ns, 4):
                        grp = segs[g0:g0 + 4]
                        S_, kS = kb.bank(pc % 3)
                        PT, kPT = PTs[pc % 3]
                        pc += 1
                        for gi, (k0, bi) in enumerate(grp):
                            kb.mm(S_[0:64, gi * 128:(gi + 1) * 128], KT[0:65, k0:k0 + 64], QT[0:65, :], True, bi is None, r=[kKT, kQT], w=[kS])
                            if bi is not None:
                                kb.mm(S_[0:64, gi * 128:(gi + 1) * 128], self.identb[0:64, 0:64], BT[0:64, bi, :], False, True, r=[kBT], w=[kS])
                        W_ = len(grp) * 128
                        kb.act(PT[0:64, 0:W_], S_[0:64, 0:W_], AF.Exp, r=[kS], w=[kPT])
                        for gi, (k0, bi) in enumerate(grp):
                            si = g0 + gi
                            kb.mm(O[0:65, 0:128], V[0:64, k0 // 64, 0:65], PT[0:64, gi * 128:(gi + 1) * 128], si == 0, si == ns - 1, r=[kV, kPT], w=[kO])
                    kb.cp('act', osb[0:65, :], O[0:65, 0:128], r=[kO], w=[kosb])
                    kb.recip(rinv[64:65, :], osb[64:65, :], r=[kosb], w=[krinv])
                    Bc, kBc = kb.bank(6 + qc % 2)
                    kb.mm(Bc[0:64, 0:128], onesf[0:65, 0:64], rinv[0:65, :], True, True, r=[konesf, krinv], w=[kBc])
                    kb.tt('dve', mix[0:64, :], osb[0:64, :], Bc[0:64, 0:128], ALU.mult, r=[kosb, kBc], w=[kmix])
                    kb.dma('sp', self.MIXE[b][h * 64:(h + 1) * 64, tok], mix[0:64, :], r=[kmix])

    return dict(even_mixer=even_mixer, na_attn=na_attn)


for _k, _v in _even_methods().items():
    setattr(Prog, _k, _v)


def _ssd_methods():
    def ssd_conv(self, l):
        kb = self.kb
        p = self.W
        i = l // 2
        kb.phase()
        bf = lambda shape: kb.sb(shape, BF16)
        cw, kcw = kb.sb([16, 5])
        cb, kcb = kb.sb([16])
        for k in range(5):
            kb.S.dma('sp', lambda e, k=k: e.dma_start(out=cw[:, :, k], in_=p['e_conv_w'][i, k].rearrange("(c p) -> p c", p=128),
                                                      allow_slow_non_contiguous=True), writes=[kcw])
        kb.S.dma('sp', lambda e: e.dma_start(out=cb, in_=p['e_conv_b'][i].rearrange("(c p) -> p c", p=128),
                                             allow_slow_non_contiguous=True), writes=[kcb])
        raws = [kb.sb([1028]) for _ in range(2)]
        acc, kacc = kb.sb([1024])
        Us = [bf([1024]) for _ in range(2)]
        tss = [bf([8, 128]) for _ in range(2)]
        it = 0
        segs = [(0, 256, True, True)] + [(256 + 1024 * s, 1024, s == 0, s == 3) for s in range(4)]
        for b in range(NB):
            for c in range(16):
                for (s0, n, first, lastseg) in segs:
                    raw, kraw = raws[it % 2]
                    U, kU = Us[it % 2]
                    tsb, ktsb = tss[it % 2]
                    it += 1
                    lo = 0 if first else 2
                    hi = 0 if lastseg else 2
                    if first:
                        kb.memset('pool', raw[:, 0:2], 0.0, w=[kraw])
                    if lastseg:
                        kb.memset('pool', raw[:, n + 2:n + 4], 0.0, w=[kraw])
                    kb.dma('sp', raw[:, 2 - lo:n + 2 + hi], self.XBCT[b][c * 128:(c + 1) * 128, s0 - lo:s0 + n + hi], w=[kraw])
                    kb.ts('dve', acc[:, 0:n], raw[:, 0:n], cw[:, c, 0:1], None, ALU.mult, r=[kraw, kcw], w=[kacc])
                    for k in range(1, 5):
                        kb.stt(acc[:, 0:n], raw[:, k:k + n], cw[:, c, k:k + 1], acc[:, 0:n], ALU.mult, ALU.add, r=[kraw, kcw, kacc], w=[kacc])
                    kb.act(U[:, 0:n], acc[:, 0:n], AF.Silu, bias=cb[:, c:c + 1], r=[kacc, kcb], w=[kU])
                    if c >= 8:
                        kb.dma('sp', self.BCT[b][(c - 8) * 128:(c - 7) * 128, s0:s0 + n], U[:, 0:n], r=[kU])
                    if c < 12:
                        nt_ = n // 128
                        pT, kpT = kb.bank(it % 2, [8, 128], BF16)
                        for tt_ in range(nt_):
                            kb.tr(pT[:, tt_, :], U[:, tt_ * 128:(tt_ + 1) * 128], self.identb, r=[kU], w=[kpT])
                        kb.cp('dve', tsb[:, 0:nt_, :], pT[:, 0:nt_, :], r=[kpT], w=[ktsb])
                        kb.dma('sp', self.XBS[b][s0:s0 + n, c * 128:(c + 1) * 128].rearrange("(t p) f -> p t f", p=128), tsb[:, 0:nt_, :], r=[ktsb])

    def ssd_scan(self, l):
        kb = self.kb
        p = self.W
        kb.phase()
        bf = lambda shape: kb.sb(shape, BF16)
        tri, ktri = kb.sb([2, 128])
        kb.dma('sp', tri, p['tri'].rearrange("d s l -> s d l"), w=[ktri])
        onesM, kon = kb.sb([128])
        kb.memset('dve', onesM, 1.0, w=[kon])
        hT, khT = kb.sb([16, 64])
        hTb, khTb = bf([16, 64])
        xss = [bf([1536]) for _ in range(2)]
        bcts = [bf([8, 128]) for _ in range(2)]
        dtas = [kb.sb([64]) for _ in range(2)]
        acs, kacs = kb.sb([16])
        Dg, kDg = kb.sb([16, 128])
        ER, kER = kb.sb([16, 128])
        CBm, kCBm = kb.sb([4, 128])
        xdt, kxdt = bf([16, 64])
        xdte, kxdte = bf([16, 64])
        segs_ = [kb.sb([128]) for _ in range(2)]
        decs = [kb.sb([128]) for _ in range(2)]
        MTs = [bf([128]) for _ in range(2)]
        Css = [bf([128]) for _ in range(2)]
        ysb, kysb = kb.sb([1024])
        dte, kdte = kb.sb([16])
        it = 0
        for b in range(NB):
            for d in range(2):
                kb.memset('dve', hT, 0.0, w=[khT])
                kb.memset('pool', hTb, 0.0, w=[khTb])
                order = [0, 1] + list(range(2, NT)) if d == 0 else [1, 0] + list(range(NT - 1, 1, -1))
                last = 127 if d == 0 else 0
                for t in order:
                    tok = slice(t * 128, (t + 1) * 128)
                    xs, kxs = xss[it % 2]
                    bct, kbct = bcts[it % 2]
                    dta, kdta = dtas[it % 2]
                    it += 1
                    kb.dma('sp', xs, self.XBS[b][tok, :], w=[kxs])
                    kb.dma('pool', bct, self.BCT[b][:, tok].rearrange("(j n) t -> n j t", n=128), w=[kbct])
                    kb.dma('sp', dta, self.DTA[b][tok, :], w=[kdta])
                    dt = dta[:, d * 16:(d + 1) * 16]
                    a = dta[:, 32 + d * 16:48 + d * 16]
                    P0, kP0 = kb.bank(0)
                    kb.mm(P0[:, 0:16], tri[:, d, :], a, True, True, r=[ktri, kdta], w=[kP0])
                    kb.cp('dve', acs, P0[:, 0:16], r=[kP0], w=[kacs])
                    kb.tt('pool', Dg, self.identf.unsqueeze(1).to_broadcast([128, 16, 128]), acs.unsqueeze(2).to_broadcast([128, 16, 128]),
                          ALU.mult, r=[kacs], w=[kDg])
                    Rb = []
                    for j in range(4):
                        Rj, kRj = kb.bank(2 + j)
                        kb.mm(Rj, onesM, Dg[:, 4 * j:4 * j + 4, :].rearrange("p h l -> p (h l)"), True, True, r=[kon, kDg], w=[kRj])
                        kb.act(ER[:, 4 * j:4 * j + 4, :].rearrange("p h l -> p (h l)"), Rj, AF.Exp, r=[kRj], w=[kER])
                        Rb.append((Rj.rearrange("p (h l) -> p h l", h=4), kRj))
                    PC, kPC = kb.bank(1)
                    for g in range(4):
                        kb.mm(PC[:, g * 128:(g + 1) * 128], bct[:, g, :], bct[:, 4 + g, :], True, True, r=[kbct], w=[kPC])
                    kb.tt('dve', CBm, PC.rearrange("p (g l) -> p g l", g=4), tri[:, d, :].unsqueeze(1).to_broadcast([128, 4, 128]), ALU.mult,
                          r=[kPC, ktri], w=[kCBm])
                    kb.tt('dve', xdt, xs[:, 0:1024].rearrange("p (h d) -> p h d", h=16), dt.unsqueeze(2).to_broadcast([128, 16, 64]), ALU.mult,
                          r=[kxs, kdta], w=[kxdt])
                    Yb = [kb.bank(6), kb.bank(7)]
                    for h in range(16):
                        g = h // 4
                        Rv, kRv = Rb[h // 4]
                        sg, ksg = segs_[h % 2]
                        dc, kdc = decs[h % 2]
                        MT, kMT = MTs[h % 2]
                        Cs, kCs = Css[h % 2]
                        kb.ts('dve', sg, Rv[:, h % 4, :], acs[:, h:h + 1], 0.0, ALU.subtract, ALU.min, r=[kRv, kacs], w=[ksg])
                        kb.act(dc, sg, AF.Exp, r=[ksg], w=[kdc])
                        kb.tt('pool', MT, dc, CBm[:, g, :], ALU.mult, r=[kdc, kCBm], w=[kMT])
                        kb.tt('pool', Cs, bct[:, 4 + g, :], ER[:, h, :], ALU.mult, r=[kbct, kER], w=[kCs])
                        Y, kY = Yb[h // 8]
                        yv = Y[:, (h % 8) * 64:(h % 8 + 1) * 64]
                        kb.mm(yv, MT, xdt[:, h, :], True, False, r=[kMT, kxdt], w=[kY])
                        kb.mm(yv, Cs, hTb[:, h, :], False, True, r=[kCs, khTb], w=[kY])
                    for j in range(2):
                        kb.cp('act', ysb[:, j * 512:(j + 1) * 512], Yb[j][0], r=[Yb[j][1]], w=[kysb])
                    kb.dma('sp', self.YD[d][b][tok, :], ysb, r=[kysb])
                    for j in range(4):
                        Rv, kRv = Rb[j]
                        kb.tt('dve', dte[:, 4 * j:4 * j + 4], Rv[:, :, last], acs[:, 4 * j:4 * j + 4], ALU.subtract, r=[kRv, kacs], w=[kdte])
                    kb.act(dte, dte, AF.Exp, r=[kdte], w=[kdte])
                    kb.tt('dve', xdte, xdt, dte.unsqueeze(2).to_broadcast([128, 16, 64]), ALU.mult, r=[kxdt, kdte], w=[kxdte])
                    for g in range(4):
                        Y, kY = Yb[g // 2]
                        kb.mm(Y[:, (g % 2) * 256:(g % 2 + 1) * 256], xs[:, 1024 + g * 128:1024 + (g + 1) * 128],
                              xdte[:, 4 * g:4 * g + 4, :].rearrange("p h d -> p (h d)"), True, True, r=[kxs, kxdte], w=[kY])
                    kb.tt('dve', hT, hT, ER[:, :, last].unsqueeze(2).to_broadcast([128, 16, 64]), ALU.mult, r=[khT, kER], w=[khT])
                    for j in range(2):
                        hv = hT[:, 8 * j:8 * j + 8, :].rearrange("p h d -> p (h d)")
                        kb.tt('dve', hv, hv, Yb[j][0], ALU.add, r=[khT, Yb[j][1]], w=[khT])
                    kb.cp('pool', hTb, hT, r=[khT], w=[khTb])

    def ssd_gate(self, l):
        kb = self.kb
        p = self.W
        i = l // 2
        kb.phase()
        bf = lambda shape: kb.sb(shape, BF16)
        dk, kdk = kb.sb([16])
        kb.dma('sp', dk, p['e_d_skip'][i].partition_broadcast(128), w=[kdk])
        gn, kgn = kb.sb([D])
        kb.dma('sp', gn, p['e_gnorm_w'][i].partition_broadcast(128), w=[kgn])
        yfs = [kb.sb([D]) for _ in range(2)]
        ybs = [kb.sb([D]) for _ in range(2)]
        xss = [bf([D]) for _ in range(2)]
        zss = [bf([D]) for _ in range(2)]
        tmp, ktmp = kb.sb([D])
        sm, ksm = kb.sb([8])
        yn, kyn = bf([D])
        stg, kstg = bf([8, 128])
        for gt in range(2 * NT):
            b, t = divmod(gt, NT)
            tok = slice(t * 128, (t + 1) * 128)
            yf, kyf = yfs[gt % 2]
            yb, kyb = ybs[gt % 2]
            xs, kxs = xss[gt % 2]
            zs, kzs = zss[gt % 2]
            kb.dma('sp', yf, self.YD[0][b][tok, :], w=[kyf])
            kb.dma('sp', yb, self.YD[1][b][tok, :], w=[kyb])
            kb.dma('pool', xs, self.XBS[b][tok, 0:1024], w=[kxs])
            kb.dma('pool', zs, self.ZS[b][tok, :], w=[kzs])
            kb.tt('pool', yf, yf, yb, ALU.add, r=[kyf, kyb], w=[kyf])
            kb.tt('dve', tmp.rearrange("p (h d) -> p h d", h=16), xs.rearrange("p (h d) -> p h d", h=16),
                  dk.unsqueeze(2).to_broadcast([128, 16, 64]), ALU.mult, r=[kxs, kdk], w=[ktmp])
            kb.tt('dve', yf, yf, tmp, ALU.add, r=[kyf, ktmp], w=[kyf])
            kb.tt('dve', yf, yf, zs, ALU.mult, r=[kyf, kzs], w=[kyf])
            kb.act(tmp, yf, AF.Square, accum=sm[:, 0:1], r=[kyf], w=[ktmp, ksm])
            self.rstd_from_ssq(sm[:, 0:1], sm[:, 0:1], 1024, ksm)
            kb.stt(yn, yf, sm[:, 0:1], gn, ALU.mult, ALU.mult, r=[kyf, ksm, kgn], w=[kyn])
            pT, kpT = kb.bank(gt % 2, [8, 128], BF16)
            for c in range(8):
                kb.tr(pT[:, c, :], yn[:, c * 128:(c + 1) * 128], self.identb, r=[kyn], w=[kpT])
            kb.cp('act', stg, pT, r=[kpT], w=[kstg])
            kb.dma('sp', self.MIXE[b][512:1536, tok].rearrange("(c p) t -> p c t", p=128), stg, r=[kstg])

    return dict(ssd_conv=ssd_conv, ssd_scan=ssd_scan, ssd_gate=ssd_gate)


for _k, _v in _ssd_methods().items():
    setattr(Prog, _k, _v)


def kernel(**inputs):
    shared = shared_inputs(inputs)
    x = np.asarray(inputs['x'], np.float32)
    ctx = np.asarray(inputs['ctx'], np.float32)
    xin = [np.ascontiguousarray(np.concatenate([ctx[2 * c:2 * c + 2], x[2 * c:2 * c + 2]], axis=1)) for c in range(8)]
    host = [host_inputs(inputs, c) for c in range(8)]
    out = None
    for l in range(DEPTH):
        last = l == DEPTH - 1
        prog = Prog({'steps': [(l, 'mix'), (l, 'moe')], 'debug_x': not last, 'only_layer': l})
        nc = prog.build()
        sl = {}
        for k, v in shared.items():
            if k not in prog.din:
                continue
            if k in PER_LAYER:
                sl[k] = np.ascontiguousarray(v[l:l + 1])
            elif k in PER_PAIR:
                sl[k] = np.ascontiguousarray(v[l // 2:l // 2 + 1])
            else:
                sl[k] = v
        in_maps = []
        for c in range(8):
            m = dict(sl)
            for k, v in host[c].items():
                if k in prog.din:
                    m[k] = v
            m['xin'] = xin[c]
            in_maps.append(m)
        res = run_bass_kernel_spmd(nc, in_maps, core_ids=list(range(8)))
        if last:
            out = np.concatenate([np.asarray(r['y'], np.float32) for r in res.results], axis=0)
        else:
            xin = [np.ascontiguousarray(np.asarray(r['xres'], np.float32)) for r in res.results]
    return out
```

```python
import contextlib
import numpy as np
import concourse.bass as bass
import concourse.mybir as mybir
from concourse.bass_utils import run_bass_kernel_spmd

F32 = mybir.dt.float32
BF16 = mybir.dt.bfloat16
I32 = mybir.dt.int32
AF = mybir.ActivationFunctionType
ALU = mybir.AluOpType
AX = mybir.AxisListType

ENGS = ['pe', 'act', 'dve', 'pool', 'sp']
DMAQ = ('sp', 'act', 'pool')
NDS = 10

D = 1024
NB = 2
LC = 256
LL = 4096
LT = LC + LL
NT = LT // 128
DEPTH = 4
ALPHA = (2.0 * DEPTH) ** 0.25
EPS = 1e-6


class _Op:
    __slots__ = ('eng', 'fn', 'waits', 'dma', 'slot', 'val', 'sig', 'prev')

    def __init__(self, eng, fn, dma):
        self.eng = eng
        self.fn = fn
        self.dma = dma
        self.waits = []
        self.slot = None
        self.val = None
        self.sig = False
        self.prev = None


class Sched:
    def __init__(self, nc):
        self.nc = nc
        self.ops = {e: [] for e in ENGS}
        self.lastw = {}
        self.reads = {}
        self.dcnt = {e: [0] * NDS for e in DMAQ}
        self.dlast = {e: [None] * NDS for e in DMAQ}
        self.di = {e: 0 for e in DMAQ}
        self.lastc = {e: None for e in ENGS}

    def _add(self, eng, fn, reads, writes, dma):
        op = _Op(eng, fn, dma)
        if dma:
            s = self.di[eng] % NDS
            self.di[eng] += 1
            self.dcnt[eng][s] += 16
            op.slot = s
            op.val = self.dcnt[eng][s]
            op.sig = True
            op.prev = self.dlast[eng][s]
            self.dlast[eng][s] = op
        else:
            self.lastc[eng] = op
        seen = set()
        for k in reads:
            w = self.lastw.get(k)
            if w is not None and id(w) not in seen:
                seen.add(id(w))
                self._dep(op, w, 'raw')
        for k in writes:
            w = self.lastw.get(k)
            if w is not None and id(w) not in seen:
                seen.add(id(w))
                self._dep(op, w, 'waw')
            for r in self.reads.get(k, {}).values():
                if id(r) not in seen:
                    seen.add(id(r))
                    self._dep(op, r, 'war')
        for k in writes:
            self.lastw[k] = op
            self.reads[k] = {}
        for k in reads:
            rk = self.reads.setdefault(k, {})
            rk[(eng, id(op)) if dma else eng] = op
        self.ops[eng].append(op)
        return op

    def _dep(self, op, d, kind):
        if d is op:
            return
        if (not d.dma) and (not op.dma) and d.eng == op.eng and (kind != 'raw' or op.eng == 'pe'):
            return
        d.sig = True
        op.waits.append(d)

    def op(self, eng, fn, reads=(), writes=()):
        return self._add(eng, fn, reads, writes, False)

    def dma(self, eng, fn, reads=(), writes=()):
        return self._add(eng, fn, reads, writes, True)

    def barrier(self):
        marks = []
        for e in ENGS:
            if self.lastc[e] is not None:
                self.lastc[e].sig = True
                marks.append(self.lastc[e])
        for q in DMAQ:
            for s in range(NDS):
                if self.dlast[q][s] is not None:
                    marks.append(self.dlast[q][s])
        for e in ENGS:
            op = _Op(e, None, False)
            op.waits = [m for m in marks if not (m.eng == e and not m.dma)]
            self.ops[e].append(op)
        self.lastw = {}
        self.reads = {}

    def emit(self):
        nc = self.nc
        self.barrier()
        with contextlib.ExitStack() as st:
            esem = {e: st.enter_context(nc.semaphore('s_' + e)) for e in ENGS}
            dsem = {e: [st.enter_context(nc.semaphore('d_%s_%d' % (e, i))) for i in range(NDS)] for e in DMAQ}
            for e in ENGS:
                cnt = 0
                for op in self.ops[e]:
                    if (not op.dma) and op.sig:
                        cnt += 1
                        op.val = cnt
            block = st.enter_context(nc.Block())

            def semof(d):
                return dsem[d.eng][d.slot] if d.dma else esem[d.eng]

            def run(e, eng):
                waited = {}
                for op in self.ops[e]:
                    ws = {}
                    lst = list(op.waits)
                    if op.dma and op.prev is not None:
                        lst.append(op.prev)
                    for d in lst:
                        s = semof(d)
                        if ws.get(id(s), (0, None))[0] < d.val:
                            ws[id(s)] = (d.val, s)
                    for key, (v, s) in ws.items():
                        if waited.get(key, 0) < v:
                            eng.wait_ge(s, v)
                            waited[key] = v
                    if op.fn is None:
                        continue
                    ins = op.fn(eng)
                    if op.sig:
                        ins.then_inc(semof(op), 16 if op.dma else 1)

            @block.sync
            def _(eng):
                run('sp', eng)

            @block.tensor
            def _(eng):
                run('pe', eng)

            @block.scalar
            def _(eng):
                run('act', eng)

            @block.vector
            def _(eng):
                run('dve', eng)

            @block.gpsimd
            def _(eng):
                run('pool', eng)


class KB:
    def __init__(self, nc, st):
        self.nc = nc
        self.S = Sched(nc)
        self.AR = 50688
        self.arena = st.enter_context(nc.sbuf_tensor("arena", [128, self.AR], F32))
        self.banks = [st.enter_context(nc.psum_tensor("bank%d" % i, [128, 512], F32)) for i in range(8)]
        self.off = 0
        self.ptop = self.AR
        self.uid = 0

    def phase(self):
        self.S.barrier()
        self.off = 0

    def _view(self, a, n32, shape, dtype):
        v = self.arena[:, a:a + n32]
        if dtype == BF16:
            v = v.bitcast(BF16)
        elif dtype == I32:
            v = v.bitcast(I32)
        if len(shape) == 2:
            v = v.rearrange("p (a b) -> p a b", a=shape[0])
        elif len(shape) == 3:
            v = v.rearrange("p (a b c) -> p a b c", a=shape[0], b=shape[1])
        return v

    def sb(self, shape, dtype=F32, persist=False):
        n = int(np.prod(shape))
        n32 = n if dtype != BF16 else (n + 1) // 2
        if persist:
            self.ptop -= n32
            a = self.ptop
        else:
            a = self.off
            self.off += n32
        assert self.off <= self.ptop, "SBUF arena overflow %d > %d" % (self.off, self.ptop)
        self.uid += 1
        return self._view(a, n32, shape, dtype), 'sb%d' % self.uid

    def bank(self, i, shape=None, dtype=F32):
        v = self.banks[i][:]
        if dtype == BF16:
            v = v.bitcast(BF16)
        if shape is not None and len(shape) == 2:
            v = v[:, 0:shape[0] * shape[1]].rearrange("p (a b) -> p a b", a=shape[0])
        elif shape is not None and len(shape) == 1:
            v = v[:, 0:shape[0]]
        return v, 'bank%d' % i

    def dma(self, q, out, in_, r=(), w=()):
        return self.S.dma(q, lambda e: e.dma_start(out=out, in_=in_), reads=r, writes=w)

    def mm(self, out, lhsT, rhs, start, stop, r=(), w=()):
        return self.S.op('pe', lambda e: e.matmul(out, lhsT=lhsT, rhs=rhs, start=start, stop=stop), reads=r, writes=w)

    def tr(self, out, in_, ident, r=(), w=()):
        return self.S.op('pe', lambda e: e.transpose(out=out, in_=in_, identity=ident), reads=r, writes=w)

    def act(self, out, in_, func, bias=None, scale=1.0, accum=None, r=(), w=(), eng='act'):
        def f(e):
            kw = {}
            if bias is not None:
                kw['bias'] = bias
            if accum is not None:
                kw['accum_out'] = accum
            return e.activation(out=out, in_=in_, func=func, scale=scale, **kw)
        return self.S.op('act', f, reads=r, writes=w)

    def ts(self, eng, out, in0, s1, s2, op0, op1=None, accum=None, r=(), w=()):
        def f(e):
            kw = {}
            if accum is not None:
                kw['accum_out'] = accum
            if op1 is None:
                return e.tensor_scalar(out, in0, s1, None, op0, **kw)
            return e.tensor_scalar(out, in0, s1, s2, op0, op1, **kw)
        return self.S.op(eng, f, reads=r, writes=w)

    def tt(self, eng, out, in0, in1, op, r=(), w=()):
        return self.S.op(eng, lambda e: e.tensor_tensor(out, in0, in1, op), reads=r, writes=w)

    def stt(self, out, in0, scalar, in1, op0, op1, r=(), w=()):
        return self.S.op('dve', lambda e: e.scalar_tensor_tensor(out, in0, scalar, in1, op0, op1), reads=r, writes=w)

    def cp(self, eng, out, in_, r=(), w=()):
        if eng == 'act':
            return self.S.op('act', lambda e: e.copy(out=out, in_=in_), reads=r, writes=w)
        return self.S.op(eng, lambda e: e.tensor_copy(out=out, in_=in_), reads=r, writes=w)

    def red(self, out, in_, op, r=(), w=(), axis=None):
        ax = AX.X if axis is None else axis
        return self.S.op('dve', lambda e: e.tensor_reduce(out, in_, ax, op), reads=r, writes=w)

    def recip(self, out, in_, r=(), w=()):
        return self.S.op('dve', lambda e: e.reciprocal(out, in_), reads=r, writes=w)

    def memset(self, eng, ap, c, w=()):
        return self.S.op(eng, lambda e: e.memset(ap, c), writes=w)


def P(name):
    return name


class LSel:
    def __init__(self, ap, base):
        self.ap = ap
        self.base = base

    def __getitem__(self, key):
        if isinstance(key, tuple):
            return self.ap[(key[0] - self.base,) + tuple(key[1:])]
        return self.ap[key - self.base]


PER_LAYER = ('ada_w', 'ada_b', 'ln_g', 'ln_b', 'moe_wr', 'moe_br')
PER_PAIR = ('o_w_in', 'o_w_out', 'o_mla_q_norm', 'o_mla_kv_norm', 'o_w_uq', 'o_w_ukv', 'o_gqa_q_norm', 'o_gqa_k_norm',
            'e_w_in', 'e_w_out', 'e_conv_w', 'e_conv_b', 'e_dt_bias', 'e_a_log', 'e_d_skip', 'e_gnorm_w', 'nabias')


class Prog:
    def __init__(self, cfg):
        self.cfg = cfg
        nc = bass.Bass("TRN2", target_bir_lowering=False)
        self.nc = nc
        self.st = contextlib.ExitStack()
        self.kb = KB(nc, self.st)
        self.din = {}
        self.uid = 0

    def inp(self, name, shape, dtype=F32):
        only = self.cfg.get('only_layer')
        shape = list(shape)
        base = None
        if only is not None and name in PER_LAYER:
            shape[0] = 1
            base = only
        elif only is not None and name in PER_PAIR:
            if (name[0] == 'o') != (only % 2 == 1):
                return None
            shape[0] = 1
            base = only // 2
        elif only is not None and name in ('rope',) and only % 2 == 0:
            return None
        elif only is not None and name in ('tri',) and only % 2 == 1:
            return None
        t = self.nc.dram_tensor(name, shape, dtype, kind="ExternalInput").ap()
        self.din[name] = t
        return LSel(t, base) if base is not None else t

    def scratch(self, name, shape, dtype=F32, out=False):
        return self.nc.dram_tensor(name, list(shape), dtype, kind="ExternalOutput" if out else "Internal").ap()

    def prologue(self):
        kb = self.kb
        cin = self.inp("cin", [4, D])
        ada_w = self.inp("ada_w", [DEPTH, D, 6 * D])
        ada_b = self.inp("ada_b", [DEPTH, 6 * D])
        identd = self.inp("ident", [128, 128])
        self.modd = self.scratch("modd", [DEPTH, 4, 6 * D])
        self.identf, kif = kb.sb([128], F32, persist=True)
        self.identb, kib = kb.sb([128], BF16, persist=True)
        self.modT, kmt = kb.sb([DEPTH * 4, 48], F32, persist=True)
        self.k_modT = kmt
        kb.dma('sp', self.identf, identd, w=[kif])
        kb.cp('dve', self.identb, self.identf, r=[kif], w=[kib])
        scr, kscr = kb.sb([4, 8])
        sc, ksc = kb.sb([8, 4])
        kb.dma('sp', scr, cin.rearrange("b (p kc) -> p b kc", kc=8), w=[kscr])
        kb.act(sc.rearrange("p kc b -> p b kc"), scr, AF.Silu, r=[kscr], w=[ksc])
        adab, kab = kb.sb([6 * D])
        adabT, kabT = kb.sb([48])
        modsb, kms = kb.sb([6 * D])
        aws = [kb.sb([8, 512]) for _ in range(2)]
        for l in sorted(set(l_ for (l_, _w) in self.cfg['steps'])):
            kb.dma('sp', adab[0:4], ada_b[l].partition_broadcast(4), w=[kab])
            kb.S.dma('sp', lambda e, l=l: e.dma_start(out=adabT, in_=ada_b[l].rearrange("(c p) -> p c", p=128),
                                                      allow_slow_non_contiguous=True), writes=[kabT])
            for nt in range(12):
                aw, kaw = aws[nt % 2]
                kb.dma('sp' if nt % 2 == 0 else 'pool', aw,
                       ada_w[l][:, nt * 512:(nt + 1) * 512].rearrange("(p kc) n -> p kc n", kc=8), w=[kaw])
                b0, kb0 = kb.bank(0)
                b1, kb1 = kb.bank(1)
                for kc in range(8):
                    kb.mm(b0[0:4, :], sc[:, kc, :], aw[:, kc, :], kc == 0, kc == 7, r=[ksc, kaw], w=[kb0])
                kb.tt('dve', modsb[0:4, nt * 512:(nt + 1) * 512], b0[0:4, :], adab[0:4, nt * 512:(nt + 1) * 512], ALU.add,
                      r=[kb0, kab], w=[kms])
                for q in range(4):
                    for kc in range(8):
                        kb.mm(b1[:, q * 4:(q + 1) * 4], aw[:, kc, q * 128:(q + 1) * 128], sc[:, kc, :], kc == 0, kc == 7,
                              r=[ksc, kaw], w=[kb1])
                b1v = b1[:, 0:16].rearrange("p (q b) -> p q b", q=4)
                for b in range(4):
                    kb.tt('dve', self.modT[:, l * 4 + b, nt * 4:(nt + 1) * 4], b1v[:, :, b], adabT[:, nt * 4:(nt + 1) * 4], ALU.add,
                          r=[kb1, kabT], w=[kmt])
            for j in (1, 4):
                v = self.modT[:, l * 4:(l + 1) * 4, j * 8:(j + 1) * 8]
                kb.ts('dve', v, v, 1.0, None, ALU.add, r=[kmt], w=[kmt])
            kb.dma('sp', self.modd[l], modsb[0:4, :], r=[kms], w=['modd'])

    def mod_col(self, l, bsel, j, kc):
        c = j * 8 + kc
        return self.modT[:, l * 4 + bsel, c:c + 1]

    def mt_tile(self, src, src_key, l, bsel, j0, hT, hT_key, xt, xt_key, bk):
        kb = self.kb
        kb.dma('sp', xt, src, r=[src_key], w=[xt_key])
        pv = [kb.bank(bk[0], [4, 128]), kb.bank(bk[1], [4, 128])]
        for kc in range(8):
            bv, bkey = pv[kc // 4]
            kb.tr(bv[:, kc % 4, :], xt[:, kc * 128:(kc + 1) * 128], self.identf, r=[xt_key], w=[bkey])
        for kc in range(8):
            bv, bkey = pv[kc // 4]
            sc_ = self.mod_col(l, bsel, j0 + 1, kc)
            sh_ = self.mod_col(l, bsel, j0, kc)
            if kc % 2 == 0:
                kb.act(hT[:, kc, :], bv[:, kc % 4, :], AF.Identity, bias=sh_, scale=sc_, r=[bkey, self.k_modT], w=[hT_key])
            else:
                kb.ts('dve', hT[:, kc, :], bv[:, kc % 4, :], sc_, sh_, ALU.mult, ALU.add, r=[bkey, self.k_modT], w=[hT_key])

    def ln_tile(self, u, ku, out, kout, lng, lnb, kln, st6, kst, mv, kmv):
        kb = self.kb
        kb.S.op('dve', lambda e: e.bn_stats(st6[:, 0, :], u[:, 0:512]), reads=[ku], writes=[kst])
        kb.S.op('dve', lambda e: e.bn_stats(st6[:, 1, :], u[:, 512:1024]), reads=[ku], writes=[kst])
        kb.S.op('dve', lambda e: e.bn_aggr(mv[:, 0:2], st6), reads=[kst], writes=[kmv])
        kb.ts('dve', mv[:, 2:3], mv[:, 1:2], EPS, None, ALU.add, r=[kmv], w=[kmv])
        kb.act(mv[:, 3:4], mv[:, 2:3], AF.Sqrt, r=[kmv], w=[kmv])
        kb.recip(mv[:, 4:5], mv[:, 3:4], r=[kmv], w=[kmv])
        kb.ts('dve', u, u, mv[:, 0:1], mv[:, 4:5], ALU.subtract, ALU.mult, r=[ku, kmv], w=[ku])
        kb.tt('pool', u, u, lng, ALU.mult, r=[ku, kln], w=[ku])
        kb.tt('dve', out, u, lnb, ALU.add, r=[ku, kln], w=[kout])

    @staticmethod
    def tile_of(gt):
        b, t = divmod(gt, NT)
        return b, t, (2 if t < 2 else b)

    def moe_phase(self, l, Xs, Xd, last):
        kb = self.kb
        p = self.W
        tiles = [gt for gt in range(2 * NT) if not (last and (gt % NT) < 2)]
        ngrp = 4
        per = (len(tiles) + ngrp - 1) // ngrp
        groups = [tiles[i * per:(i + 1) * per] for i in range(ngrp)]
        kb.phase()
        wr, kwr = kb.sb([8, 36], BF16)
        br, kbr = kb.sb([36])
        kb.dma('pool', wr, p['moe_wr'][l].rearrange("(kc p) n -> p kc n", p=128), w=[kwr])
        kb.dma('sp', br, p['moe_br'][l].partition_broadcast(128), w=[kbr])
        base_off = kb.off
        for grp in groups:
            kb.S.barrier()
            kb.off = base_off
            ng = len(grp)
            hT, khT = kb.sb([8, per * 128], BF16)
            G, kG = kb.sb([per, 32])
            acc, kacc = kb.sb([per, D])
            grp_off = kb.off
            xts = [kb.sb([D]) for _ in range(2)]
            rs = [kb.sb([64]) for _ in range(2)]
            for i, gt in enumerate(grp):
                b, t, bsel = self.tile_of(gt)
                xt, kxt = xts[i % 2]
                khi = khT + '_%d' % i
                self.mt_tile(Xs[b, t * 128:(t + 1) * 128, :], ('X', gt), l, bsel, 3, hT[:, :, i * 128:(i + 1) * 128], khi,
                             xt, kxt, (0, 1))
                pb, kpb = kb.bank(2 + i % 2)
                for kc in range(8):
                    kb.mm(pb[:, 0:36], hT[:, kc, i * 128:(i + 1) * 128], wr[:, kc, :], kc == 0, kc == 7, r=[khi, kwr], w=[kpb])
                R, kR = rs[i % 2]
                L = R[:, 0:36]
                kb.tt('dve', L, pb[:, 0:36], br, ALU.add, r=[kpb, kbr], w=[kR])
                gmax = R[:, 36:37]
                kb.red(gmax, L[:, 0:4], ALU.max, r=[kR], w=[kR])
                ngmax = R[:, 37:38]
                kb.ts('dve', ngmax, gmax, -1.0, None, ALU.mult, r=[kR], w=[kR])
                gsum = R[:, 38:39]
                kb.act(R[:, 40:44], L[:, 0:4], AF.Exp, bias=ngmax, accum=gsum, r=[kR], w=[kR])
                ggate = R[:, 39:40]
                kb.recip(ggate, gsum, r=[kR], w=[kR])
                ohg = R[:, 44:48]
                kb.ts('dve', ohg, L[:, 0:4], gmax, None, ALU.is_equal, r=[kR], w=[kR])
                ein = R[:, 48:56]
                kb.ts('dve', ein, L[:, 4:12], ohg[:, 0:1], None, ALU.mult, r=[kR], w=[kR])
                for g in range(1, 4):
                    kb.stt(ein, L[:, 4 + 8 * g:12 + 8 * g], ohg[:, g:g + 1], ein, ALU.mult, ALU.add, r=[kR], w=[kR])
                m1 = R[:, 56:57]
                kb.red(m1, ein, ALU.max, r=[kR], w=[kR])
                Grow = G[:, i, :]
                kGi = kG + '_%d' % i
                oh1 = Grow[:, 0:8]
                oh2 = Grow[:, 8:16]
                ein2 = Grow[:, 16:24]
                kb.ts('dve', oh1, ein, m1, None, ALU.is_equal, r=[kR], w=[kGi])
                kb.stt(ein2, oh1, -1.0e30, ein, ALU.mult, ALU.add, r=[kR, kGi], w=[kGi])
                m2 = R[:, 57:58]
                kb.red(m2, ein2, ALU.max, r=[kGi], w=[kR])
                kb.ts('dve', oh2, ein2, m2, None, ALU.is_equal, r=[kR, kGi], w=[kGi])
                dd = R[:, 58:59]
                kb.tt('dve', dd, m2, m1, ALU.subtract, r=[kR], w=[kR])
                ed = R[:, 59:60]
                kb.act(ed, dd, AF.Exp, r=[kR], w=[kR])
                den = R[:, 60:61]
                kb.ts('dve', den, ed, 1.0, None, ALU.add, r=[kR], w=[kR])
                w1 = R[:, 61:62]
                kb.recip(w1, den, r=[kR], w=[kR])
                g1 = R[:, 62:63]
                kb.tt('dve', g1, w1, ggate, ALU.mult, r=[kR], w=[kR])
                g2 = R[:, 63:64]
                kb.tt('dve', g2, g1, ed, ALU.mult, r=[kR], w=[kR])
                ge = Grow[:, 24:32]
                kb.ts('dve', ge, oh1, g1, None, ALU.mult, r=[kR, kGi], w=[kGi])
                kb.stt(ein, oh2, g2, ge, ALU.mult, ALU.add, r=[kR, kGi], w=[kR])
                for g in range(4):
                    kb.ts('dve', Grow[:, 8 * g:8 * g + 8], ein, ohg[:, g:g + 1], None, ALU.mult, r=[kR], w=[kGi])
            kb.S.barrier()
            kb.off = grp_off
            wb = [(kb.sb([8, 512], BF16), kb.sb([8, 512], BF16), kb.sb([4, D], BF16)) for _ in range(2)]
            sils = [kb.sb([512]) for _ in range(2)]
            aTs = [kb.sb([4, 512], BF16) for _ in range(2)]
            chunks = [(s, min(4, ng - s)) for s in range(0, ng, 4)]
            cc = 0
            oc = 0
            for e in range(32):
                (w1b, kw1), (w3b, kw3), (w2b, kw2) = wb[e % 2]
                kb.dma('pool', w1b, p['moe_w1'][l, e].rearrange("(kc p) n -> p kc n", p=128), w=[kw1])
                kb.dma('pool', w3b, p['moe_w3'][l, e].rearrange("(kc p) n -> p kc n", p=128), w=[kw3])
                kb.dma('pool', w2b, p['moe_w2'][l, e].rearrange("(kc p) n -> p kc n", p=128), w=[kw2])
                for (s, n) in chunks:
                    N = n * 128
                    aT, kaT = aTs[cc % 2]
                    cc += 1
                    hks = [khT + '_%d' % i for i in range(s, s + n)]
                    for hc in range(4):
                        A, kA = kb.bank((hc % 2) * 2)
                        B, kB = kb.bank((hc % 2) * 2 + 1)
                        for kc in range(8):
                            kb.mm(A[:, 0:N], w1b[:, kc, hc * 128:(hc + 1) * 128], hT[:, kc, s * 128:s * 128 + N], kc == 0, kc == 7,
                                  r=[kw1] + hks, w=[kA])
                        for kc in range(8):
                            kb.mm(B[:, 0:N], w3b[:, kc, hc * 128:(hc + 1) * 128], hT[:, kc, s * 128:s * 128 + N], kc == 0, kc == 7,
                                  r=[kw3] + hks, w=[kB])
                        sl, ksl = sils[hc % 2]
                        kb.act(sl[:, 0:N], A[:, 0:N], AF.Silu, r=[kA], w=[ksl])
                        kb.tt('dve', aT[:, hc, 0:N], sl[:, 0:N], B[:, 0:N], ALU.mult, r=[ksl, kB], w=[kaT + '_%d' % hc])
                    kas = [kaT + '_%d' % hc for hc in range(4)]
                    for ti in range(n):
                        i = s + ti
                        for nt in range(2):
                            O, kO = kb.bank(4 + oc % 4)
                            oc += 1
                            for hc in range(4):
                                kb.mm(O, aT[:, hc, ti * 128:(ti + 1) * 128], w2b[:, hc, nt * 512:(nt + 1) * 512], hc == 0, hc == 3,
                                      r=[kw2] + kas, w=[kO])
                            av = acc[:, i, nt * 512:(nt + 1) * 512]
                            ka = kacc + '_%d_%d' % (i, nt)
                            gcol = G[:, i, e:e + 1]
                            if e == 0:
                                kb.ts('dve', av, O, gcol, None, ALU.mult, r=[kO, kG + '_%d' % i], w=[ka])
                            else:
                                kb.stt(av, O, gcol, av, ALU.mult, ALU.add, r=[kO, kG + '_%d' % i, ka], w=[ka])
            kb.S.barrier()
            kb.off = grp_off
            lng, kln = kb.sb([D])
            lnb, _ = kb.sb([D])
            kb.dma('sp', lng, p['ln_g'][l, 1].partition_broadcast(128), w=[kln])
            kb.dma('sp', lnb, p['ln_b'][l, 1].partition_broadcast(128), w=[kln])
            gts = []
            for bsel in range(3):
                gt_, kgt = kb.sb([D])
                kb.dma('sp', gt_, self.modd[l, bsel, 5 * D:6 * D].partition_broadcast(128), r=['modd'], w=[kgt])
                gts.append((gt_, kgt))
            x1s = [kb.sb([D]) for _ in range(2)]
            outs = [kb.sb([D]) for _ in range(2)]
            sts = [kb.sb([2, 6]) for _ in range(2)]
            mvs = [kb.sb([8]) for _ in range(2)]
            for i, gt in enumerate(grp):
                b, t, bsel = self.tile_of(gt)
                x1, kx1 = x1s[i % 2]
                kb.dma('sp', x1, Xs[b, t * 128:(t + 1) * 128, :], r=[('X', gt)], w=[kx1])
                gtile, kgt = gts[bsel]
                av = acc[:, i, :]
                kas = [kacc + '_%d_%d' % (i, nt) for nt in range(2)]
                kb.tt('pool', av, av, gtile, ALU.mult, r=kas + [kgt], w=kas)
                kb.stt(x1, x1, ALPHA, av, ALU.mult, ALU.add, r=[kx1] + kas, w=[kx1])
                o, ko = outs[i % 2]
                st6, kst = sts[i % 2]
                mv, kmv = mvs[i % 2]
                self.ln_tile(x1, kx1, o, ko, lng, lnb, kln, st6, kst, mv, kmv)
                dst, kd = Xd(gt)
                kb.dma('sp', dst, o, r=[ko], w=[kd])


    def declare_weights(self):
        W = {}
        W['ln_g'] = self.inp('ln_g', [DEPTH, 2, D])
        W['ln_b'] = self.inp('ln_b', [DEPTH, 2, D])
        W['moe_wr'] = self.inp('moe_wr', [DEPTH, D, 36])
        W['moe_br'] = self.inp('moe_br', [DEPTH, 36])
        lays = sorted(set(l_ for (l_, w_) in self.cfg['steps'] if w_ == 'moe'))
        for nm in ('moe_w1p', 'moe_w3p', 'moe_w2p'):
            W[nm] = {l_: self.inp('%s_%d' % (nm, l_), [4096, 4096]) for l_ in lays}
        W['striu'] = self.inp('striu', [128, 128])
        W['thr68'] = self.inp('thr68', [68])
        W['jidx'] = self.inp('jidx', [168])
        W['pidx'] = self.inp('pidx', [128, 1])
        self.XSL = self.scratch('XSL', [168 * 128, D], BF16)
        self.YB = self.scratch('YB', [168 * 128, D], F32)
        self.HM = self.scratch('HM', [2 * LT, D], BF16)
        for nm, shp in (('o_w_in', [2, D, 1440]), ('o_w_out', [2, D, D]), ('o_mla_q_norm', [2, 384]), ('o_mla_kv_norm', [2, 256]),
                        ('o_w_uq', [2, 384, 768]), ('o_w_ukv', [2, 256, 1024]), ('o_gqa_q_norm', [2, 64]), ('o_gqa_k_norm', [2, 64]),
                        ('rope', [32, 128, 96])):
            W[nm] = self.inp(nm, shp)
        self.W = W
        sc = lambda nm, shp: [self.scratch('%s%d' % (nm, b), shp, BF16) for b in range(NB)]
        self.QTm = sc('QTm', [8, 97, LT])
        self.KTm = sc('KTm', [8, 97, LT])
        self.Vm = sc('Vm', [LT, 8, 65])
        self.QTg = sc('QTg', [8, 65, LT])
        self.KTg = sc('KTg', [2, 65, LT])
        self.Vg = sc('Vg', [LT, 2, 65])
        self.MIXT = sc('MIXT', [16, 64, LT])
        for nm, shp in (('e_w_in', [2, D, 4640]), ('e_w_out', [2, 1536, D]), ('e_conv_w', [2, 5, 2048]), ('e_conv_b', [2, 2048]),
                        ('e_dt_bias', [2, 2, 16]), ('e_a_log', [2, 2, 16]), ('e_d_skip', [2, 16]), ('e_gnorm_w', [2, D]),
                        ('nabias', [2, 8, 64, 45, 128]), ('tri', [2, 128, 128])):
            W[nm] = self.inp(nm, shp)
        self.QTn = sc('QTn', [8, 65, LT])
        self.KTn = sc('KTn', [8, 65, LT])
        self.Vn = sc('Vn', [LT, 8, 65])
        self.MIXE = sc('MIXE', [1536, LT])
        self.XBS = sc('XBS', [LT, 1536])
        self.BCT = sc('BCT', [1024, LT])
        self.ZS = sc('ZS', [LT, D])
        self.XBCT = [self.scratch('XBCT%d' % b, [2048, LT], F32) for b in range(NB)]
        self.DTA = [self.scratch('DTA%d' % b, [LT, 64], F32) for b in range(NB)]
        self.YD = [[self.scratch('YD%d_%d' % (d, b), [LT, D], F32) for b in range(NB)] for d in range(2)]

    def build(self):
        cfg = self.cfg
        self.declare_weights()
        self.xin = self.inp('xin', [NB, LT, D])
        dbg = cfg.get('debug_x', False)
        self.Y = self.scratch('y', [NB, LL, D], out=(not dbg))
        self.X = self.scratch('xres', [NB, LT, D], out=dbg)
        self.prologue()
        steps = cfg['steps']
        first = True
        for (l, what) in steps:
            src = self.xin if first else self.X
            first = False
            last = (l == DEPTH - 1)
            if what == 'mix':
                def Xd1(gt):
                    b, t = divmod(gt, NT)
                    return self.X[b, t * 128:(t + 1) * 128, :], ('X', gt)
                if l % 2 == 1:
                    self.odd_prep(l, src)
                    self.odd_attn(l)
                    self.odd_out(l, src, Xd1)
                else:
                    self.even_mixer(l, src, Xd1)
            if what == 'moe':
                if last and not dbg:
                    def Xd(gt):
                        b, t = divmod(gt, NT)
                        return self.Y[b, (t - 2) * 128:(t - 1) * 128, :], ('Y', gt)
                else:
                    def Xd(gt):
                        b, t = divmod(gt, NT)
                        return self.X[b, t * 128:(t + 1) * 128, :], ('X', gt)
                self.moe_sparse(l, src, Xd, last)
        self.kb.S.emit()
        self.st.close()
        return self.nc


def host_inputs(inputs, core):
    f = lambda a: np.ascontiguousarray(np.asarray(a, dtype=np.float32))
    b0 = 2 * core
    m = {}
    m['cin'] = f(np.stack([inputs['c'][b0], inputs['c'][b0 + 1], inputs['c_ctx'], inputs['c_ctx']], 0))
    m['ident'] = np.eye(128, dtype=np.float32)
    return m


def shared_inputs(inputs):
    f = lambda a: np.ascontiguousarray(np.asarray(a, dtype=np.float32))
    m = {}
    L_ = inputs['moe_w1'].shape[0]
    for l_ in range(L_):
        m['moe_w1p_%d' % l_] = np.ascontiguousarray(f(inputs['moe_w1'][l_]).reshape(32, 8, 128, 512).transpose(0, 2, 1, 3)).reshape(4096, 4096)
        m['moe_w3p_%d' % l_] = np.ascontiguousarray(f(inputs['moe_w3'][l_]).reshape(32, 8, 128, 512).transpose(0, 2, 1, 3)).reshape(4096, 4096)
        m['moe_w2p_%d' % l_] = np.ascontiguousarray(f(inputs['moe_w2'][l_]).reshape(32, 4, 128, 1024).transpose(0, 2, 1, 3)).reshape(4096, 4096)
    m['striu'] = np.triu(np.ones((128, 128), np.float32), k=1)
    m['thr68'] = (np.arange(68) * 128).astype(np.float32)
    m['jidx'] = np.arange(168).astype(np.float32)
    m['pidx'] = np.arange(128).astype(np.float32).reshape(128, 1)
    for k in ('ada_w', 'ada_b', 'ln_g', 'ln_b', 'o_w_in', 'o_w_out', 'o_mla_q_norm', 'o_mla_kv_norm',
              'o_w_uq', 'o_w_ukv', 'o_gqa_q_norm', 'o_gqa_k_norm'):
        m[k] = f(inputs[k])
    m['rope'] = rope_tables()
    for k in ('e_w_in', 'e_w_out', 'e_conv_w', 'e_conv_b', 'e_dt_bias', 'e_a_log', 'e_d_skip', 'e_gnorm_w'):
        m[k] = f(inputs[k])
    m['nabias'] = np.stack([na_bias_tables(np.asarray(inputs['e_rpb'][i], np.float32)) for i in range(2)], 0)
    tri = np.triu(np.ones((128, 128), np.float32))
    m['tri'] = np.ascontiguousarray(np.stack([tri, tri.T], 0))
    m['moe_wr'] = f(np.concatenate([inputs['moe_wg'], inputs['moe_we']], axis=-1))
    m['moe_br'] = f(np.concatenate([inputs['moe_bg'], inputs['moe_be']], axis=-1))
    return m


MLA_SCALE = 96 ** -0.5
GQA_SCALE = 64 ** -0.5


def rope_tables():
    out = np.zeros((32, 128, 96), np.float32)
    pos = np.arange(LL)
    row, col = pos // 64, pos % 64
    for (half, o) in ((16, 0), (32, 32)):
        inv = np.power(np.float32(10000.0), -np.arange(0, half, 2, dtype=np.float32) / np.float32(half)).astype(np.float32)
        nf = half // 2
        ar = row.astype(np.float32)[:, None] * inv
        ac = col.astype(np.float32)[:, None] * inv
        cosv = np.concatenate([np.cos(ar), np.cos(ac)], 1).astype(np.float32)
        sinv = np.concatenate([np.sin(ar), np.sin(ac)], 1).astype(np.float32)
        out[:, :, o:o + 2 * nf] = cosv.reshape(32, 128, 2 * nf)
        out[:, :, o + 2 * nf:o + 4 * nf] = sinv.reshape(32, 128, 2 * nf)
    return out


def _odd_methods():
    def rope(self, dst, src, H, half, cos, sin, lat, kd, ks, ktab):
        kb = self.kb
        nf = half // 2
        if not lat:
            kb.cp('dve', dst, src, r=[ks], w=[kd])
            return
        t1, k1 = self.rt[0]
        t2, k2 = self.rt[1]
        for rc in range(2):
            v1 = src[:, :, rc * half:rc * half + nf]
            v2 = src[:, :, rc * half + nf:(rc + 1) * half]
            o1 = dst[:, :, rc * half:rc * half + nf]
            o2 = dst[:, :, rc * half + nf:(rc + 1) * half]
            c = cos[:, rc * nf:(rc + 1) * nf].unsqueeze(1).to_broadcast([128, H, nf])
            s = sin[:, rc * nf:(rc + 1) * nf].unsqueeze(1).to_broadcast([128, H, nf])
            a = t1[:, 0:H * nf].rearrange("p (h f) -> p h f", h=H)
            b_ = t2[:, 0:H * nf].rearrange("p (h f) -> p h f", h=H)
            kb.tt('dve', a, v1, c, ALU.mult, r=[ks, ktab], w=[k1])
            kb.tt('pool', b_, v2, s, ALU.mult, r=[ks, ktab], w=[k2])
            kb.tt('dve', o1, a, b_, ALU.subtract, r=[k1, k2], w=[kd])
            kb.tt('dve', a, v1, s, ALU.mult, r=[ks, ktab], w=[k1])
            kb.tt('pool', b_, v2, c, ALU.mult, r=[ks, ktab], w=[k2])
            kb.tt('dve', o2, a, b_, ALU.add, r=[k1, k2], w=[kd])

    def rstd_from_ssq(self, out, ssq, n, k):
        kb = self.kb
        kb.ts('dve', out, ssq, 1.0 / n, EPS, ALU.mult, ALU.add, r=[k], w=[k])
        kb.act(out, out, AF.Sqrt, r=[k], w=[k])
        kb.recip(out, out, r=[k], w=[k])

    def odd_prep(self, l, Xs):
        kb = self.kb
        p = self.W
        i = l // 2
        kb.phase()
        bf = lambda shape: kb.sb(shape, BF16)
        win, kwin = bf([8, 1440])
        wuq, kwuq = bf([3, 768])
        wukv, kwukv = bf([2, 1024])
        kb.dma('pool', win, p['o_w_in'][i].rearrange("(kc p) n -> p kc n", p=128), w=[kwin])
        kb.dma('pool', wuq, p['o_w_uq'][i].rearrange("(kc p) n -> p kc n", p=128), w=[kwuq])
        kb.dma('pool', wukv, p['o_w_ukv'][i].rearrange("(kc p) n -> p kc n", p=128), w=[kwukv])
        gq, kgq = kb.sb([384])
        gkv, kgkv = kb.sb([256])
        ggq, kggq = kb.sb([64])
        ggk, kggk = kb.sb([64])
        kb.dma('sp', gq, p['o_mla_q_norm'][i].partition_broadcast(128), w=[kgq])
        kb.dma('sp', gkv, p['o_mla_kv_norm'][i].partition_broadcast(128), w=[kgkv])
        kb.dma('sp', ggq, p['o_gqa_q_norm'][i].partition_broadcast(128), w=[kggq])
        kb.dma('sp', ggk, p['o_gqa_k_norm'][i].partition_broadcast(128), w=[kggk])
        kb.ts('dve', ggq, ggq, GQA_SCALE, None, ALU.mult, r=[kggq], w=[kggq])
        ropet, krope = kb.sb([32, 96])
        kb.dma('sp', ropet, p['rope'].rearrange("t p c -> p t c"), w=[krope])
        kmr, kkmr = kb.sb([16])
        ones, kones = bf([LT])
        kb.memset('pool', ones, 1.0, w=[kones])
        xts = [kb.sb([D]) for _ in range(2)]
        hTs = [bf([8, 128]) for _ in range(2)]
        pj, kpj = kb.sb([1440])
        sm, ksm = kb.sb([64])
        cqn, kcqn = bf([384])
        cqnT, kcqnT = bf([3, 128])
        ckvn, kckvn = bf([256])
        ckvnT, kckvnT = bf([2, 128])
        q, kq = kb.sb([8, 96])
        kv, kkv = kb.sb([8, 128])
        tmp, ktmp = kb.sb([1024])
        gn, kgn = kb.sb([10, 64])
        self.rt = [kb.sb([256]) for _ in range(2)]
        qa, kqa = bf([8, 98])
        ka, kka = bf([8, 98])
        va, kva = bf([8, 66])
        qga, kqga = bf([8, 66])
        kga, kkga = bf([2, 66])
        vga, kvga = bf([2, 66])
        stq, kstq = bf([8, 128])
        stk, kstk = bf([8, 128])
        stg, kstg = bf([8, 128])
        stkg, kstkg = bf([2, 128])
        krw, kkrw = bf([LT])
        for b in range(NB):
            kb.memset('dve', kmr, 0.0, w=[kkmr])
            for t in range(NT):
                lat = t >= 2
                bsel = b if lat else 2
                gt = b * NT + t
                xt, kxt = xts[t % 2]
                hT, khT = hTs[t % 2]
                self.mt_tile(Xs[b, t * 128:(t + 1) * 128, :], ('X', gt), l, bsel, 0, hT, khT, xt, kxt, (0, 1))
                for nt, (c0, c1) in enumerate(((0, 512), (512, 1024), (1024, 1440))):
                    pb, kpb = kb.bank(2 + nt)
                    for kc in range(8):
                        kb.mm(pb[:, 0:c1 - c0], hT[:, kc, :], win[:, kc, c0:c1], kc == 0, kc == 7, r=[khT, kwin], w=[kpb])
                    kb.cp('act', pj[:, c0:c1], pb[:, 0:c1 - c0], r=[kpb], w=[kpj])
                if lat:
                    tb = ropet[:, t - 2, :]
                    cm, sm_, cg, sg = tb[:, 0:16], tb[:, 16:32], tb[:, 32:64], tb[:, 64:96]
                else:
                    cm = sm_ = cg = sg = None
                kb.act(tmp[:, 0:384], pj[:, 0:384], AF.Square, accum=sm[:, 0:1], r=[kpj], w=[ktmp, ksm])
                self.rstd_from_ssq(sm[:, 0:1], sm[:, 0:1], 384, ksm)
                kb.stt(cqn, pj[:, 0:384], sm[:, 0:1], gq, ALU.mult, ALU.mult, r=[kpj, ksm, kgq], w=[kcqn])
                pT, kpT = kb.bank(5, [8, 128], BF16)
                for c in range(3):
                    kb.tr(pT[:, c, :], cqn[:, c * 128:(c + 1) * 128], self.identb, r=[kcqn], w=[kpT])
                kb.cp('dve', cqnT, pT[:, 0:3, :], r=[kpT], w=[kcqnT])
                qf = q.rearrange("p h d -> p (h d)")
                for nt, (c0, c1) in enumerate(((0, 512), (512, 768))):
                    pb, kpb = kb.bank(2 + nt)
                    for c in range(3):
                        kb.mm(pb[:, 0:c1 - c0], cqnT[:, c, :], wuq[:, c, c0:c1], c == 0, c == 2, r=[kcqnT, kwuq], w=[kpb])
                    kb.act(qf[:, c0:c1], pb[:, 0:c1 - c0], AF.Copy, scale=MLA_SCALE, r=[kpb], w=[kq])
                kb.cp('dve', qa[:, :, 0:64], q[:, :, 0:64], r=[kq], w=[kqa])
                self.rope(qa[:, :, 64:96], q[:, :, 64:96], 8, 16, cm, sm_, lat, kqa, kq, krope)
                t3 = tmp[:, 0:768].rearrange("p (h d) -> p h d", h=8)
                kb.tt('pool', t3, q, q, ALU.mult, r=[kq], w=[ktmp])
                kb.red(sm[:, 8:16], t3, ALU.add, r=[ktmp], w=[ksm])
                kb.act(sm[:, 8:16], sm[:, 8:16], AF.Sqrt, r=[ksm], w=[ksm])
                kb.ts('dve', qa[:, :, 96], sm[:, 8:16], -1.0, None, ALU.mult, r=[ksm], w=[kqa])
                kb.act(tmp[:, 0:256], pj[:, 896:1152], AF.Square, accum=sm[:, 1:2], r=[kpj], w=[ktmp, ksm])
                self.rstd_from_ssq(sm[:, 1:2], sm[:, 1:2], 256, ksm)
                kb.stt(ckvn, pj[:, 896:1152], sm[:, 1:2], gkv, ALU.mult, ALU.mult, r=[kpj, ksm, kgkv], w=[kckvn])
                for c in range(2):
                    kb.tr(pT[:, 4 + c, :], ckvn[:, c * 128:(c + 1) * 128], self.identb, r=[kckvn], w=[kpT])
                kb.cp('dve', ckvnT, pT[:, 4:6, :], r=[kpT], w=[kckvnT])
                kvf = kv.rearrange("p h d -> p (h d)")
                for nt in range(2):
                    pb, kpb = kb.bank(2 + nt)
                    for c in range(2):
                        kb.mm(pb, ckvnT[:, c, :], wukv[:, c, nt * 512:(nt + 1) * 512], c == 0, c == 1, r=[kckvnT, kwukv], w=[kpb])
                    kb.cp('act', kvf[:, nt * 512:(nt + 1) * 512], pb, r=[kpb], w=[kkv])
                kb.cp('dve', ka[:, :, 0:64], kv[:, :, 0:64], r=[kkv], w=[kka])
                kb.cp('pool', va[:, :, 0:64], kv[:, :, 64:128], r=[kkv], w=[kva])
                kb.memset('pool', va[:, :, 64:65], 1.0, w=[kva])
                kr = gn[:, 9, 0:32]
                self.rope(kr.unsqueeze(1), pj[:, 1152:1184].unsqueeze(1), 1, 16, cm, sm_, lat, kgn, kpj, krope)
                kb.cp('dve', ka[:, :, 64:96], kr.unsqueeze(1).to_broadcast([128, 8, 32]), r=[kgn], w=[kka])
                t4 = tmp[:, 0:512].rearrange("p (h d) -> p h d", h=8)
                kb.tt('pool', t4, kv[:, :, 0:64], kv[:, :, 0:64], ALU.mult, r=[kkv], w=[ktmp])
                kb.red(sm[:, 16:24], t4, ALU.add, r=[ktmp], w=[ksm])
                kb.act(tmp[:, 512:544], pj[:, 1152:1184], AF.Square, accum=sm[:, 2:3], r=[kpj], w=[ktmp, ksm])
                kb.ts('dve', sm[:, 16:24], sm[:, 16:24], sm[:, 2:3], None, ALU.add, r=[ksm], w=[ksm])
                kb.tt('dve', kmr[:, 0:8], kmr[:, 0:8], sm[:, 16:24], ALU.max, r=[ksm, kkmr], w=[kkmr])
                for (src, H, gtab, kg, dst, kdst, s0) in ((pj[:, 384:896], 8, ggq, kggq, qga, kqga, 24),
                                                          (pj[:, 1184:1312], 2, ggk, kggk, kga, kkga, 40)):
                    sv = src.rearrange("p (h d) -> p h d", h=H)
                    tv = tmp[:, 0:H * 64].rearrange("p (h d) -> p h d", h=H)
                    kb.tt('pool', tv, sv, sv, ALU.mult, r=[kpj], w=[ktmp])
                    ss = sm[:, s0:s0 + H]
                    kb.red(ss, tv, ALU.add, r=[ktmp], w=[ksm])
                    self.rstd_from_ssq(ss, ss, 64, ksm)
                    gv_ = gn[:, 0:H, :]
                    kb.tt('dve', gv_, sv, ss.unsqueeze(2).to_broadcast([128, H, 64]), ALU.mult, r=[kpj, ksm], w=[kgn])
                    kb.tt('dve', gv_, gv_, gtab.unsqueeze(1).to_broadcast([128, H, 64]), ALU.mult, r=[kgn, kg], w=[kgn])
                    self.rope(dst[:, :, 0:64], gv_, H, 32, cg, sg, lat, kdst, kgn, krope)
                    kb.tt('pool', tv, gv_, gv_, ALU.mult, r=[kgn], w=[ktmp])
                    ss2 = sm[:, s0 + 8:s0 + 8 + H]
                    kb.red(ss2, tv, ALU.add, r=[ktmp], w=[ksm])
                    if H == 8:
                        kb.act(ss2, ss2, AF.Sqrt, r=[ksm], w=[ksm])
                        kb.ts('dve', dst[:, :, 64], ss2, -1.0, None, ALU.mult, r=[ksm], w=[kdst])
                    else:
                        kb.tt('dve', kmr[:, 8:10], kmr[:, 8:10], ss2, ALU.max, r=[ksm, kkmr], w=[kkmr])
                gvv = pj[:, 1312:1440].rearrange("p (h d) -> p h d", h=2)
                kb.cp('dve', vga[:, :, 0:64], gvv, r=[kpj], w=[kvga])
                kb.memset('pool', vga[:, :, 64:65], 1.0, w=[kvga])
                tok = slice(t * 128, (t + 1) * 128)
                for (srcb, ksrc, H, dd, stg_, kst_, dst_d, bkn) in (
                        (qa, kqa, 8, 97, stq, kstq, self.QTm[b], 6), (ka, kka, 8, 96, stk, kstk, self.KTm[b], 7),
                        (qga, kqga, 8, 65, stg, kstg, self.QTg[b], 6), (kga, kkga, 2, 64, stkg, kstkg, self.KTg[b], 7)):
                    pT2, kpT2 = kb.bank(bkn, [8, 128], BF16)
                    for h in range(H):
                        kb.tr(pT2[0:dd, h, :], srcb[:, h, 0:dd], self.identb, r=[ksrc], w=[kpT2])
                    kb.cp('act', stg_[0:dd, 0:H, :], pT2[0:dd, 0:H, :], r=[kpT2], w=[kst_])
                    kb.dma('sp', dst_d[:, 0:dd, tok].rearrange("h d t -> d h t"), stg_[0:dd, 0:H, :], r=[kst_])
                kb.dma('sp', self.Vm[b][tok, :, :], va[:, :, 0:65], r=[kva])
                kb.dma('sp', self.Vg[b][tok, :, :], vga[:, :, 0:65], r=[kvga])
            pk, kpk = kb.bank(5)
            kb.tr(pk[0:10, 0:128], kmr[:, 0:10], self.identf, r=[kkmr], w=[kpk])
            kb.red(sm[0:10, 60:61], pk[0:10, 0:128], ALU.max, r=[kpk], w=[ksm])
            kb.act(sm[0:10, 60:61], sm[0:10, 60:61], AF.Sqrt, r=[ksm], w=[ksm])
            kb.ts('dve', krw[0:10, :], ones[0:10, :], sm[0:10, 60:61], None, ALU.mult, r=[ksm, kones], w=[kkrw])
            for h in range(8):
                kb.dma('sp', self.KTm[b][h, 96:97, :], krw[h:h + 1, :], r=[kkrw])
            for h in range(2):
                kb.dma('sp', self.KTg[b][h, 64:65, :], krw[8 + h:9 + h, :], r=[kkrw])

    return dict(rope=rope, rstd_from_ssq=rstd_from_ssq, odd_prep=odd_prep)


for _k, _v in _odd_methods().items():
    setattr(Prog, _k, _v)


def _attn_methods():
    def attn_core(self, jobs, bias_fn=None):
        kb = self.kb
        bf = lambda shape: kb.sb(shape, BF16)
        KTs = [bf([LT]) for _ in range(2)]
        Vs = [bf([NT, 66]) for _ in range(2)]
        QTs = [bf([512]) for _ in range(2)]
        PTs = [bf([512]) for _ in range(3)]
        osbs = [kb.sb([512]) for _ in range(2)]
        rinv, krinv = kb.sb([512])
        onesf, konesf = kb.sb([64])
        mixs = [bf([512]) for _ in range(2)]
        kb.memset('dve', onesf, 1.0, w=[konesf])
        kb.memset('dve', rinv, 0.0, w=[krinv])
        qc = 0
        pc = 0
        lc = 0
        lastKV = None
        for ji, job in enumerate(jobs):
            dk, nkt = job['dk'], job['nkt']
            if job.get('load', True):
                KT, kKT = KTs[lc % 2]
                V, kV = Vs[lc % 2]
                lc += 1
                kb.dma('sp', KT[0:dk, 0:nkt * 128], job['KT'][:, 0:nkt * 128], w=[kKT])
                kb.S.dma('pool', lambda e, V=V, job=job, nkt=nkt: e.dma_start(
                    out=V[:, 0:nkt, 0:65], in_=job['V'][0:nkt * 128, :].rearrange("(t p) c -> p t c", p=128)), writes=[kV])
                lastKV = (KT, kKT, V, kV)
            else:
                KT, kKT, V, kV = lastKV
            for (QTd, N, nk, outd) in job['qblocks']:
                QT, kQT = QTs[qc % 2]
                O, kO = kb.bank(4 + qc % 2)
                osb, kosb = osbs[qc % 2]
                mix, kmix = mixs[qc % 2]
                qc += 1
                kb.dma('sp', QT[0:dk, 0:N], QTd, w=[kQT])
                for kt in range(nk):
                    S_, kS = kb.bank(pc % 3)
                    PT, kPT = PTs[pc % 3]
                    pc += 1
                    kb.mm(S_[:, 0:N], KT[0:dk, kt * 128:(kt + 1) * 128], QT[0:dk, 0:N], True, True, r=[kKT, kQT], w=[kS])
                    kb.act(PT[:, 0:N], S_[:, 0:N], AF.Exp, r=[kS], w=[kPT])
                    kb.mm(O[0:65, 0:N], V[:, kt, 0:65], PT[:, 0:N], kt == 0, kt == nk - 1, r=[kV, kPT], w=[kO])
                kb.cp('act', osb[0:65, 0:N], O[0:65, 0:N], r=[kO], w=[kosb])
                kb.recip(rinv[64:65, 0:N], osb[64:65, 0:N], r=[kosb], w=[krinv])
                Bc, kBc = kb.bank(6 + qc % 2)
                kb.mm(Bc[0:64, 0:N], onesf[0:65, 0:64], rinv[0:65, 0:N], True, True, r=[konesf, krinv], w=[kBc])
                kb.tt('dve', mix[0:64, 0:N], osb[0:64, 0:N], Bc[0:64, 0:N], ALU.mult, r=[kosb, kBc], w=[kmix])
                kb.dma('sp', outd, mix[0:64, 0:N], r=[kmix])

    def odd_attn(self, l):
        kb = self.kb
        kb.phase()
        ctxq = l < DEPTH - 1
        jobs = []
        for b in range(NB):
            for h in range(16):
                if h < 8:
                    KT, V, QT, dk = self.KTm[b][h], self.Vm[b][:, h, :], self.QTm[b][h], 97
                    load = True
                else:
                    g = (h - 8) // 4
                    KT, V, QT, dk = self.KTg[b][g], self.Vg[b][:, g, :], self.QTg[b][h - 8], 65
                    load = ((h - 8) % 4 == 0)
                qbs = []
                if ctxq:
                    qbs.append((QT[:, 0:256], 256, 2, self.MIXT[b][h, :, 0:256]))
                for qb in range(8):
                    s = 256 + qb * 512
                    qbs.append((QT[:, s:s + 512], 512, NT, self.MIXT[b][h, :, s:s + 512]))
                jobs.append(dict(KT=KT, V=V, dk=dk, nkt=NT, qblocks=qbs, load=load))
        self.attn_core(jobs)

    def mix_out(self, l, Xs, Xd, wout_ap, nk, kparts, mix_loader, last_ctx_skip):
        kb = self.kb
        p = self.W
        kb.phase()
        wo, kwo = kb.sb([nk, D], BF16)
        kb.dma('pool', wo[0:kparts], wout_ap, w=[kwo])
        lng, kln = kb.sb([D])
        lnb, _ = kb.sb([D])
        kb.dma('sp', lng, p['ln_g'][l, 0].partition_broadcast(128), w=[kln])
        kb.dma('sp', lnb, p['ln_b'][l, 0].partition_broadcast(128), w=[kln])
        gts = []
        for bsel in range(3):
            gt_, kgt = kb.sb([D])
            kb.dma('sp', gt_, self.modd[l, bsel, 2 * D:3 * D].partition_broadcast(128), r=['modd'], w=[kgt])
            gts.append((gt_, kgt))
        mts = [kb.sb([nk, 128], BF16) for _ in range(2)]
        x1s = [kb.sb([D]) for _ in range(2)]
        ys = [kb.sb([D]) for _ in range(2)]
        outs = [kb.sb([D]) for _ in range(2)]
        sts = [kb.sb([2, 6]) for _ in range(2)]
        mvs = [kb.sb([8]) for _ in range(2)]
        ii = 0
        for gt in range(2 * NT):
            b, t, bsel = self.tile_of(gt)
            if last_ctx_skip and t < 2:
                continue
            mt, kmt_ = mts[ii % 2]
            x1, kx1 = x1s[ii % 2]
            y, ky = ys[ii % 2]
            o, ko = outs[ii % 2]
            st6, kst = sts[ii % 2]
            mv, kmv = mvs[ii % 2]
            mix_loader(gt, mt, kmt_)
            kb.dma('sp', x1, Xs[b, t * 128:(t + 1) * 128, :], r=[('X', gt)], w=[kx1])
            gtile, kgt = gts[bsel]
            for nt in range(2):
                Y, kY = kb.bank(2 * (ii % 2) + nt)
                for c in range(nk):
                    kb.mm(Y, mt[0:kparts, c, :], wo[0:kparts, c, nt * 512:(nt + 1) * 512], c == 0, c == nk - 1, r=[kmt_, kwo], w=[kY])
                kb.tt('dve', y[:, nt * 512:(nt + 1) * 512], Y, gtile[:, nt * 512:(nt + 1) * 512], ALU.mult, r=[kY, kgt], w=[ky])
            kb.stt(x1, x1, ALPHA, y, ALU.mult, ALU.add, r=[kx1, ky], w=[kx1])
            self.ln_tile(x1, kx1, o, ko, lng, lnb, kln, st6, kst, mv, kmv)
            dst, kd = Xd(gt)
            kb.dma('sp', dst, o, r=[ko], w=[kd])
            ii += 1

    def odd_out(self, l, Xs, Xd):
        kb = self.kb
        i = l // 2

        def loader(gt, mt, kmt_):
            b, t = divmod(gt, NT)
            kb.dma('sp', mt[0:64], self.MIXT[b][:, :, t * 128:(t + 1) * 128].rearrange("h d t -> d h t"), w=[kmt_])
        self.mix_out(l, Xs, Xd, self.W['o_w_out'][i].rearrange("(h d) n -> d h n", d=64), 16, 64, loader, l == DEPTH - 1)

    return dict(attn_core=attn_core, odd_attn=odd_attn, mix_out=mix_out, odd_out=odd_out)


for _k, _v in _attn_methods().items():
    setattr(Prog, _k, _v)


NA_SCALE = 64 ** -0.5


def na_bias_tables(rpb):
    out = np.full((8, 5, 9, 64, 128), -30000.0, np.float32)
    for cls, m in enumerate((2, 0, 1, 30, 31)):
        R0 = min(max(2 * m - 4, 0), 55)
        for qi in range(128):
            r, j = 2 * m + qi // 64, qi % 64
            r0 = min(max(r - 4, 0), 56)
            c0 = min(max(j - 8, 0), 48)
            for kr in range(r0, r0 + 8):
                seg = kr - R0
                cols = np.arange(c0, c0 + 16)
                out[:, cls, seg, cols, qi] = rpb[:, kr - r + 7, cols - j + 15]
    return np.ascontiguousarray(out.transpose(0, 3, 1, 2, 4)).reshape(8, 64, 45, 128)


def _even_methods():
    def even_mixer(self, l, Xs, Xd):
        kb = self.kb
        p = self.W
        i = l // 2
        bf = lambda shape: kb.sb(shape, BF16)
        ctxq = True
        kb.phase()
        win, kwin = bf([8, 4640])
        kb.dma('pool', win, p['e_w_in'][i].rearrange("(kc p) n -> p kc n", p=128), w=[kwin])
        dtb, kdtb = kb.sb([32])
        kb.dma('sp', dtb, p['e_dt_bias'][i].rearrange("a b -> (a b)").partition_broadcast(128), w=[kdtb])
        Aneg, kA = kb.sb([32])
        kb.dma('sp', Aneg, p['e_a_log'][i].rearrange("a b -> (a b)").partition_broadcast(128), w=[kA])
        kb.act(Aneg, Aneg, AF.Exp, r=[kA], w=[kA])
        kb.ts('dve', Aneg, Aneg, -1.0, None, ALU.mult, r=[kA], w=[kA])
        kmr, kkmr = kb.sb([8])
        ones, kones = bf([LT])
        kb.memset('pool', ones, 1.0, w=[kones])
        krw, kkrw = bf([LT])
        xts = [kb.sb([D]) for _ in range(2)]
        hT4, khT4 = bf([8, 512])
        ev, kev = kb.sb([1536])
        sm, ksm = kb.sb([64])
        tmp, ktmp = kb.sb([512])
        qa, kqa = bf([8, 66])
        ka, kka = bf([8, 66])
        va, kva = bf([8, 66])
        zs, kzs = bf([D])
        dts, kdts = kb.sb([64])
        stq, kstq = bf([8, 128])
        stk, kstk = bf([8, 128])
        xo, kxo = kb.sb([512])
        for b in range(NB):
            kb.memset('dve', kmr, 0.0, w=[kkmr])
            for (t0, n) in [(0, 2)] + [(2 + 4 * s, 4) for s in range(8)]:
                N = n * 128
                for ti in range(n):
                    t = t0 + ti
                    bsel = b if t >= 2 else 2
                    xt, kxt = xts[ti % 2]
                    self.mt_tile(Xs[b, t * 128:(t + 1) * 128, :], ('X', b * NT + t), l, bsel, 0, hT4[:, :, ti * 128:(ti + 1) * 128],
                                 khT4 + '_%d' % ti, xt, kxt, (0, 1))
                hks = [khT4 + '_%d' % ti for ti in range(n)]
                for c in range(16):
                    pb, kpb = kb.bank(2 + c % 2)
                    for kc in range(8):
                        kb.mm(pb[:, 0:N], win[:, kc, 2560 + c * 128:2560 + (c + 1) * 128], hT4[:, kc, 0:N], kc == 0, kc == 7,
                              r=[kwin] + hks, w=[kpb])
                    kb.cp('act' if c % 2 == 0 else 'dve', xo[:, 0:N], pb[:, 0:N], r=[kpb], w=[kxo])
                    kb.dma('sp', self.XBCT[b][c * 128:(c + 1) * 128, t0 * 128:t0 * 128 + N], xo[:, 0:N], r=[kxo])
                for ti in range(n):
                    t = t0 + ti
                    tok = slice(t * 128, (t + 1) * 128)
                    hk = [khT4 + '_%d' % ti]
                    hTt = hT4[:, :, ti * 128:(ti + 1) * 128]
                    for nt in range(3):
                        pb, kpb = kb.bank(4 + nt)
                        for kc in range(8):
                            kb.mm(pb, hTt[:, kc, :], win[:, kc, nt * 512:(nt + 1) * 512], kc == 0, kc == 7, r=[kwin] + hk, w=[kpb])
                        if nt == 0:
                            kb.act(ev[:, 0:512], pb, AF.Copy, scale=NA_SCALE, r=[kpb], w=[kev])
                        else:
                            kb.cp('act', ev[:, nt * 512:(nt + 1) * 512], pb, r=[kpb], w=[kev])
                    for nt in range(2):
                        pb, kpb = kb.bank(4 + nt)
                        for kc in range(8):
                            kb.mm(pb, hTt[:, kc, :], win[:, kc, 1536 + nt * 512:1536 + (nt + 1) * 512], kc == 0, kc == 7, r=[kwin] + hk, w=[kpb])
                        kb.act(zs[:, nt * 512:(nt + 1) * 512], pb, AF.Silu, r=[kpb], w=[kzs])
                    kb.dma('sp', self.ZS[b][tok, :], zs, r=[kzs])
                    pb, kpb = kb.bank(6)
                    for kc in range(8):
                        kb.mm(pb[:, 0:32], hTt[:, kc, :], win[:, kc, 4608:4640], kc == 0, kc == 7, r=[kwin] + hk, w=[kpb])
                    xr = dts[:, 0:32]
                    kb.tt('dve', xr, pb[:, 0:32], dtb, ALU.add, r=[kpb, kdtb], w=[kdts])
                    ab = sm[:, 0:32]
                    kb.stt(ab, xr, -1.0, xr, ALU.mult, ALU.max, r=[kdts], w=[ksm])
                    kb.act(ab, ab, AF.Exp, scale=-1.0, r=[ksm], w=[ksm])
                    kb.act(ab, ab, AF.Ln, bias=1.0, r=[ksm], w=[ksm])
                    kb.stt(xr, xr, 0.0, ab, ALU.max, ALU.add, r=[kdts, ksm], w=[kdts])
                    kb.tt('dve', dts[:, 32:64], xr, Aneg, ALU.mult, r=[kdts, kA], w=[kdts])
                    kb.dma('sp', self.DTA[b][tok, :], dts, r=[kdts])
                    q3 = ev[:, 0:512].rearrange("p (h d) -> p h d", h=8)
                    k3 = ev[:, 512:1024].rearrange("p (h d) -> p h d", h=8)
                    v3 = ev[:, 1024:1536].rearrange("p (h d) -> p h d", h=8)
                    t3 = tmp.rearrange("p (h d) -> p h d", h=8)
                    kb.cp('dve', qa[:, :, 0:64], q3, r=[kev], w=[kqa])
                    kb.tt('pool', t3, q3, q3, ALU.mult, r=[kev], w=[ktmp])
                    kb.red(sm[:, 32:40], t3, ALU.add, r=[ktmp], w=[ksm])
                    kb.act(sm[:, 32:40], sm[:, 32:40], AF.Sqrt, r=[ksm], w=[ksm])
                    kb.ts('dve', qa[:, :, 64], sm[:, 32:40], -1.0, None, ALU.mult, r=[ksm], w=[kqa])
                    kb.cp('dve', ka[:, :, 0:64], k3, r=[kev], w=[kka])
                    kb.tt('pool', t3, k3, k3, ALU.mult, r=[kev], w=[ktmp])
                    kb.red(sm[:, 40:48], t3, ALU.add, r=[ktmp], w=[ksm])
                    kb.tt('dve', kmr, kmr, sm[:, 40:48], ALU.max, r=[ksm, kkmr], w=[kkmr])
                    kb.cp('pool', va[:, :, 0:64], v3, r=[kev], w=[kva])
                    kb.memset('pool', va[:, :, 64:65], 1.0, w=[kva])
                    for (srcb, ksrc, dd, stg_, kst_, dst_d, bkn) in ((qa, kqa, 65, stq, kstq, self.QTn[b], 6), (ka, kka, 64, stk, kstk, self.KTn[b], 7)):
                        pT2, kpT2 = kb.bank(bkn, [8, 128], BF16)
                        for h in range(8):
                            kb.tr(pT2[0:dd, h, :], srcb[:, h, 0:dd], self.identb, r=[ksrc], w=[kpT2])
                        kb.cp('act', stg_[0:dd, :, :], pT2[0:dd, :, :], r=[kpT2], w=[kst_])
                        kb.dma('sp', dst_d[:, 0:dd, tok].rearrange("h d t -> d h t"), stg_[0:dd, :, :], r=[kst_])
                    kb.dma('sp', self.Vn[b][tok, :, :], va[:, :, 0:65], r=[kva])
            pk, kpk = kb.bank(5)
            kb.tr(pk[0:8, 0:128], kmr[:, 0:8], self.identf, r=[kkmr], w=[kpk])
            kb.red(sm[0:8, 60:61], pk[0:8, 0:128], ALU.max, r=[kpk], w=[ksm])
            kb.act(sm[0:8, 60:61], sm[0:8, 60:61], AF.Sqrt, r=[ksm], w=[ksm])
            kb.ts('dve', krw[0:8, :], ones[0:8, :], sm[0:8, 60:61], None, ALU.mult, r=[ksm, kones], w=[kkrw])
            for h in range(8):
                kb.dma('sp', self.KTn[b][h, 64:65, :], krw[h:h + 1, :], r=[kkrw])
        self.na_attn(l)
        self.ssd_conv(l)
        self.ssd_scan(l)
        self.ssd_gate(l)

        def loader(gt, mt, kmt_):
            b, t = divmod(gt, NT)
            kb.dma('sp', mt, self.MIXE[b][:, t * 128:(t + 1) * 128].rearrange("(c p) t -> p c t", p=128), w=[kmt_])
        self.mix_out(l, Xs, Xd, p['e_w_out'][i].rearrange("(c p) n -> p c n", p=128), 12, 128, loader, False)

    def na_attn(self, l):
        kb = self.kb
        p = self.W
        i = l // 2
        kb.phase()
        bf = lambda shape: kb.sb(shape, BF16)
        KTs = [bf([LT]) for _ in range(2)]
        Vs = [bf([68, 66]) for _ in range(2)]
        BTs = [bf([5 * 9, 128]) for _ in range(2)]
        QTs = [bf([128]) for _ in range(2)]
        PTs = [bf([512]) for _ in range(3)]
        osbs = [kb.sb([128]) for _ in range(2)]
        rinv, krinv = kb.sb([128])
        onesf, konesf = kb.sb([64])
        mixs = [bf([128]) for _ in range(2)]
        kb.memset('dve', onesf, 1.0, w=[konesf])
        kb.memset('dve', rinv, 0.0, w=[krinv])
        qc = 0
        pc = 0
        for b in range(NB):
            for h in range(8):
                j = b * 8 + h
                KT, kKT = KTs[j % 2]
                V, kV = Vs[j % 2]
                BT, kBT = BTs[j % 2]
                kb.dma('sp', KT[0:65, :], self.KTn[b][h], w=[kKT])
                kb.dma('pool', V[0:64, :, 0:65], self.Vn[b][:, h, :].rearrange("(t p) c -> p t c", p=64), w=[kV])
                kb.dma('pool', BT[0:64], p['nabias'][i, h], w=[kBT])
                for qt in range(NT):
                    if qt < 2:
                        segs = [(s * 64, None) for s in range(4)]
                    else:
                        m = qt - 2
                        cls = {0: 1, 1: 2, 30: 3, 31: 4}.get(m, 0)
                        R0 = min(max(2 * m - 4, 0), 55)
                        segs = [(s * 64, None) for s in range(4)] + [(256 + (R0 + s) * 64, cls * 9 + s) for s in range(9)]
                    QT, kQT = QTs[qc % 2]
                    O, kO = kb.bank(4 + qc % 2)
                    osb, kosb = osbs[qc % 2]
                    mix, kmix = mixs[qc % 2]
                    qc += 1
                    tok = slice(qt * 128, (qt + 1) * 128)
                    kb.dma('sp', QT[0:65, :], self.QTn[b][h][:, tok], w=[kQT])
                    ns = len(segs)
                    for g0 in range(0, ns, 4):
                        grp = segs[g0:g0 + 4]
                        S_, kS = kb.bank(pc % 3)
                        PT, kPT = PTs[pc % 3]
                        pc += 1
                        for gi, (k0, bi) in enumerate(grp):
                            kb.mm(S_[0:64, gi * 128:(gi + 1) * 128], KT[0:65, k0:k0 + 64], QT[0:65, :], True, bi is None, r=[kKT, kQT], w=[kS])
                            if bi is not None:
                                kb.mm(S_[0:64, gi * 128:(gi + 1) * 128], self.identb[0:64, 0:64], BT[0:64, bi, :], False, True, r=[kBT], w=[kS])
                        W_ = len(grp) * 128
                        kb.act(PT[0:64, 0:W_], S_[0:64, 0:W_], AF.Exp, r=[kS], w=[kPT])
                        for gi, (k0, bi) in enumerate(grp):
                            si = g0 + gi
                            kb.mm(O[0:65, 0:128], V[0:64, k0 // 64, 0:65], PT[0:64, gi * 128:(gi + 1) * 128], si == 0, si == ns - 1, r=[kV, kPT], w=[kO])
                    kb.cp('act', osb[0:65, :], O[0:65, 0:128], r=[kO], w=[kosb])
                    kb.recip(rinv[64:65, :], osb[64:65, :], r=[kosb], w=[krinv])
                    Bc, kBc = kb.bank(6 + qc % 2)
                    kb.mm(Bc[0:64, 0:128], onesf[0:65, 0:64], rinv[0:65, :], True, True, r=[konesf, krinv], w=[kBc])
                    kb.tt('dve', mix[0:64, :], osb[0:64, :], Bc[0:64, 0:128], ALU.mult, r=[kosb, kBc], w=[kmix])
                    kb.dma('sp', self.MIXE[b][h * 64:(h + 1) * 64, tok], mix[0:64, :], r=[kmix])

    return dict(even_mixer=even_mixer, na_attn=na_attn)


for _k, _v in _even_methods().items():
    setattr(Prog, _k, _v)


def _ssd_methods():
    def ssd_conv(self, l):
        kb = self.kb
        p = self.W
        i = l // 2
        kb.phase()
        bf = lambda shape: kb.sb(shape, BF16)
        cw, kcw = kb.sb([16, 5])
        cb, kcb = kb.sb([16])
        for k in range(5):
            kb.S.dma('sp', lambda e, k=k: e.dma_start(out=cw[:, :, k], in_=p['e_conv_w'][i, k].rearrange("(c p) -> p c", p=128),
                                                      allow_slow_non_contiguous=True), writes=[kcw])
        kb.S.dma('sp', lambda e: e.dma_start(out=cb, in_=p['e_conv_b'][i].rearrange("(c p) -> p c", p=128),
                                             allow_slow_non_contiguous=True), writes=[kcb])
        raws = [kb.sb([1028]) for _ in range(2)]
        acc, kacc = kb.sb([1024])
        Us = [bf([1024]) for _ in range(2)]
        tss = [bf([8, 128]) for _ in range(2)]
        it = 0
        segs = [(0, 256, True, True)] + [(256 + 1024 * s, 1024, s == 0, s == 3) for s in range(4)]
        for b in range(NB):
            for c in range(16):
                for (s0, n, first, lastseg) in segs:
                    raw, kraw = raws[it % 2]
                    U, kU = Us[it % 2]
                    tsb, ktsb = tss[it % 2]
                    it += 1
                    lo = 0 if first else 2
                    hi = 0 if lastseg else 2
                    if first:
                        kb.memset('pool', raw[:, 0:2], 0.0, w=[kraw])
                    if lastseg:
                        kb.memset('pool', raw[:, n + 2:n + 4], 0.0, w=[kraw])
                    kb.dma('sp', raw[:, 2 - lo:n + 2 + hi], self.XBCT[b][c * 128:(c + 1) * 128, s0 - lo:s0 + n + hi], w=[kraw])
                    kb.ts('dve', acc[:, 0:n], raw[:, 0:n], cw[:, c, 0:1], None, ALU.mult, r=[kraw, kcw], w=[kacc])
                    for k in range(1, 5):
                        kb.stt(acc[:, 0:n], raw[:, k:k + n], cw[:, c, k:k + 1], acc[:, 0:n], ALU.mult, ALU.add, r=[kraw, kcw, kacc], w=[kacc])
                    kb.act(U[:, 0:n], acc[:, 0:n], AF.Silu, bias=cb[:, c:c + 1], r=[kacc, kcb], w=[kU])
                    if c >= 8:
                        kb.dma('sp', self.BCT[b][(c - 8) * 128:(c - 7) * 128, s0:s0 + n], U[:, 0:n], r=[kU])
                    if c < 12:
                        nt_ = n // 128
                        pT, kpT = kb.bank(it % 2, [8, 128], BF16)
                        for tt_ in range(nt_):
                            kb.tr(pT[:, tt_, :], U[:, tt_ * 128:(tt_ + 1) * 128], self.identb, r=[kU], w=[kpT])
                        kb.cp('dve', tsb[:, 0:nt_, :], pT[:, 0:nt_, :], r=[kpT], w=[ktsb])
                        kb.dma('sp', self.XBS[b][s0:s0 + n, c * 128:(c + 1) * 128].rearrange("(t p) f -> p t f", p=128), tsb[:, 0:nt_, :], r=[ktsb])

    def ssd_scan(self, l):
        kb = self.kb
        p = self.W
        kb.phase()
        bf = lambda shape: kb.sb(shape, BF16)
        tri, ktri = kb.sb([2, 128])
        kb.dma('sp', tri, p['tri'].rearrange("d s l -> s d l"), w=[ktri])
        onesM, kon = kb.sb([128])
        kb.memset('dve', onesM, 1.0, w=[kon])
        hT, khT = kb.sb([16, 64])
        hTb, khTb = bf([16, 64])
        xss = [bf([1536]) for _ in range(2)]
        bcts = [bf([8, 128]) for _ in range(2)]
        dtas = [kb.sb([64]) for _ in range(2)]
        acs, kacs = kb.sb([16])
        Dg, kDg = kb.sb([16, 128])
        ER, kER = kb.sb([16, 128])
        CBm, kCBm = kb.sb([4, 128])
        xdt, kxdt = bf([16, 64])
        xdte, kxdte = bf([16, 64])
        segs_ = [kb.sb([128]) for _ in range(2)]
        decs = [kb.sb([128]) for _ in range(2)]
        MTs = [bf([128]) for _ in range(2)]
        Css = [bf([128]) for _ in range(2)]
        ysb, kysb = kb.sb([1024])
        dte, kdte = kb.sb([16])
        it = 0
        for b in range(NB):
            for d in range(2):
                kb.memset('dve', hT, 0.0, w=[khT])
                kb.memset('pool', hTb, 0.0, w=[khTb])
                order = [0, 1] + list(range(2, NT)) if d == 0 else [1, 0] + list(range(NT - 1, 1, -1))
                last = 127 if d == 0 else 0
                for t in order:
                    tok = slice(t * 128, (t + 1) * 128)
                    xs, kxs = xss[it % 2]
                    bct, kbct = bcts[it % 2]
                    dta, kdta = dtas[it % 2]
                    it += 1
                    kb.dma('sp', xs, self.XBS[b][tok, :], w=[kxs])
                    kb.dma('pool', bct, self.BCT[b][:, tok].rearrange("(j n) t -> n j t", n=128), w=[kbct])
                    kb.dma('sp', dta, self.DTA[b][tok, :], w=[kdta])
                    dt = dta[:, d * 16:(d + 1) * 16]
                    a = dta[:, 32 + d * 16:48 + d * 16]
                    P0, kP0 = kb.bank(0)
                    kb.mm(P0[:, 0:16], tri[:, d, :], a, True, True, r=[ktri, kdta], w=[kP0])
                    kb.cp('dve', acs, P0[:, 0:16], r=[kP0], w=[kacs])
                    kb.tt('pool', Dg, self.identf.unsqueeze(1).to_broadcast([128, 16, 128]), acs.unsqueeze(2).to_broadcast([128, 16, 128]),
                          ALU.mult, r=[kacs], w=[kDg])
                    Rb = []
                    for j in range(4):
                        Rj, kRj = kb.bank(2 + j)
                        kb.mm(Rj, onesM, Dg[:, 4 * j:4 * j + 4, :].rearrange("p h l -> p (h l)"), True, True, r=[kon, kDg], w=[kRj])
                        kb.act(ER[:, 4 * j:4 * j + 4, :].rearrange("p h l -> p (h l)"), Rj, AF.Exp, r=[kRj], w=[kER])
                        Rb.append((Rj.rearrange("p (h l) -> p h l", h=4), kRj))
                    PC, kPC = kb.bank(1)
                    for g in range(4):
                        kb.mm(PC[:, g * 128:(g + 1) * 128], bct[:, g, :], bct[:, 4 + g, :], True, True, r=[kbct], w=[kPC])
                    kb.tt('dve', CBm, PC.rearrange("p (g l) -> p g l", g=4), tri[:, d, :].unsqueeze(1).to_broadcast([128, 4, 128]), ALU.mult,
                          r=[kPC, ktri], w=[kCBm])
                    kb.tt('dve', xdt, xs[:, 0:1024].rearrange("p (h d) -> p h d", h=16), dt.unsqueeze(2).to_broadcast([128, 16, 64]), ALU.mult,
                          r=[kxs, kdta], w=[kxdt])
                    Yb = [kb.bank(6), kb.bank(7)]
                    for h in range(16):
                        g = h // 4
                        Rv, kRv = Rb[h // 4]
                        sg, ksg = segs_[h % 2]
                        dc, kdc = decs[h % 2]
                        MT, kMT = MTs[h % 2]
                        Cs, kCs = Css[h % 2]
                        kb.ts('dve', sg, Rv[:, h % 4, :], acs[:, h:h + 1], 0.0, ALU.subtract, ALU.min, r=[kRv, kacs], w=[ksg])
                        kb.act(dc, sg, AF.Exp, r=[ksg], w=[kdc])
                        kb.tt('pool', MT, dc, CBm[:, g, :], ALU.mult, r=[kdc, kCBm], w=[kMT])
                        kb.tt('pool', Cs, bct[:, 4 + g, :], ER[:, h, :], ALU.mult, r=[kbct, kER], w=[kCs])
                        Y, kY = Yb[h // 8]
                        yv = Y[:, (h % 8) * 64:(h % 8 + 1) * 64]
                        kb.mm(yv, MT, xdt[:, h, :], True, False, r=[kMT, kxdt], w=[kY])
                        kb.mm(yv, Cs, hTb[:, h, :], False, True, r=[kCs, khTb], w=[kY])
                    for j in range(2):
                        kb.cp('act', ysb[:, j * 512:(j + 1) * 512], Yb[j][0], r=[Yb[j][1]], w=[kysb])
                    kb.dma('sp', self.YD[d][b][tok, :], ysb, r=[kysb])
                    for j in range(4):
                        Rv, kRv = Rb[j]
                        kb.tt('dve', dte[:, 4 * j:4 * j + 4], Rv[:, :, last], acs[:, 4 * j:4 * j + 4], ALU.subtract, r=[kRv, kacs], w=[kdte])
                    kb.act(dte, dte, AF.Exp, r=[kdte], w=[kdte])
                    kb.tt('dve', xdte, xdt, dte.unsqueeze(2).to_broadcast([128, 16, 64]), ALU.mult, r=[kxdt, kdte], w=[kxdte])
                    for g in range(4):
                        Y, kY = Yb[g // 2]
                        kb.mm(Y[:, (g % 2) * 256:(g % 2 + 1) * 256], xs[:, 1024 + g * 128:1024 + (g + 1) * 128],
                              xdte[:, 4 * g:4 * g + 4, :].rearrange("p h d -> p (h d)"), True, True, r=[kxs, kxdte], w=[kY])
                    kb.tt('dve', hT, hT, ER[:, :, last].unsqueeze(2).to_broadcast([128, 16, 64]), ALU.mult, r=[khT, kER], w=[khT])
                    for j in range(2):
                        hv = hT[:, 8 * j:8 * j + 8, :].rearrange("p h d -> p (h d)")
                        kb.tt('dve', hv, hv, Yb[j][0], ALU.add, r=[khT, Yb[j][1]], w=[khT])
                    kb.cp('pool', hTb, hT, r=[khT], w=[khTb])

    def ssd_gate(self, l):
        kb = self.kb
        p = self.W
        i = l // 2
        kb.phase()
        bf = lambda shape: kb.sb(shape, BF16)
        dk, kdk = kb.sb([16])
        kb.dma('sp', dk, p['e_d_skip'][i].partition_broadcast(128), w=[kdk])
        gn, kgn = kb.sb([D])
        kb.dma('sp', gn, p['e_gnorm_w'][i].partition_broadcast(128), w=[kgn])
        yfs = [kb.sb([D]) for _ in range(2)]
        ybs = [kb.sb([D]) for _ in range(2)]
        xss = [bf([D]) for _ in range(2)]
        zss = [bf([D]) for _ in range(2)]
        tmp, ktmp = kb.sb([D])
        sm, ksm = kb.sb([8])
        yn, kyn = bf([D])
        stg, kstg = bf([8, 128])
        for gt in range(2 * NT):
            b, t = divmod(gt, NT)
            tok = slice(t * 128, (t + 1) * 128)
            yf, kyf = yfs[gt % 2]
            yb, kyb = ybs[gt % 2]
            xs, kxs = xss[gt % 2]
            zs, kzs = zss[gt % 2]
            kb.dma('sp', yf, self.YD[0][b][tok, :], w=[kyf])
            kb.dma('sp', yb, self.YD[1][b][tok, :], w=[kyb])
            kb.dma('pool', xs, self.XBS[b][tok, 0:1024], w=[kxs])
            kb.dma('pool', zs, self.ZS[b][tok, :], w=[kzs])
            kb.tt('pool', yf, yf, yb, ALU.add, r=[kyf, kyb], w=[kyf])
            kb.tt('dve', tmp.rearrange("p (h d) -> p h d", h=16), xs.rearrange("p (h d) -> p h d", h=16),
                  dk.unsqueeze(2).to_broadcast([128, 16, 64]), ALU.mult, r=[kxs, kdk], w=[ktmp])
            kb.tt('dve', yf, yf, tmp, ALU.add, r=[kyf, ktmp], w=[kyf])
            kb.tt('dve', yf, yf, zs, ALU.mult, r=[kyf, kzs], w=[kyf])
            kb.act(tmp, yf, AF.Square, accum=sm[:, 0:1], r=[kyf], w=[ktmp, ksm])
            self.rstd_from_ssq(sm[:, 0:1], sm[:, 0:1], 1024, ksm)
            kb.stt(yn, yf, sm[:, 0:1], gn, ALU.mult, ALU.mult, r=[kyf, ksm, kgn], w=[kyn])
            pT, kpT = kb.bank(gt % 2, [8, 128], BF16)
            for c in range(8):
                kb.tr(pT[:, c, :], yn[:, c * 128:(c + 1) * 128], self.identb, r=[kyn], w=[kpT])
            kb.cp('act', stg, pT, r=[kpT], w=[kstg])
            kb.dma('sp', self.MIXE[b][512:1536, tok].rearrange("(c p) t -> p c t", p=128), stg, r=[kstg])

    return dict(ssd_conv=ssd_conv, ssd_scan=ssd_scan, ssd_gate=ssd_gate)


for _k, _v in _ssd_methods().items():
    setattr(Prog, _k, _v)


def _moe_sparse_methods():
    def moe_sparse(self, l, Xs, Xd, last):
        kb = self.kb
        p = self.W
        bf = lambda shape: kb.sb(shape, BF16)
        tiles = [gt for gt in range(2 * NT) if not (last and (gt % NT) < 2)]
        ntl = len(tiles)
        NBLK = (2 * ntl * 128 + 32 * 127) // 128
        NSLOT = NBLK * 128
        XSL, YB, HM = self.XSL, self.YB, self.HM
        kb.phase()
        OH, kOH = bf([ntl, 64])
        PG, kPG = kb.sb([ntl, 4])
        DI, kDI = kb.sb([ntl, 2], I32)
        DF, kDF = kb.sb([ntl, 2])
        carry, kcarry = kb.sb([32])
        WI, kWI = kb.sb([NBLK], I32)
        base_off = kb.off
        wr, kwr = bf([8, 36])
        br, kbr = kb.sb([36])
        kb.dma('pool', wr, p['moe_wr'][l].rearrange("(kc p) n -> p kc n", p=128), w=[kwr])
        kb.dma('sp', br, p['moe_br'][l].partition_broadcast(128), w=[kbr])
        su_f, ksuf = kb.sb([128])
        su, ksu = bf([128])
        onesb, konesb = bf([128])
        kb.dma('sp', su_f, p['striu'], w=[ksuf])
        kb.cp('dve', su, su_f, r=[ksuf], w=[ksu])
        kb.memset('pool', onesb, 1.0, w=[konesb])
        kb.memset('dve', carry, 0.0, w=[kcarry])
        mods = []
        for bsel in range(3):
            shb, ksh = kb.sb([D])
            scb, ksc_ = kb.sb([D])
            kb.dma('sp', shb, self.modd[l, bsel, 3 * D:4 * D].partition_broadcast(128), r=['modd'], w=[ksh])
            kb.dma('sp', scb, self.modd[l, bsel, 4 * D:5 * D].partition_broadcast(128), r=['modd'], w=[ksc_])
            kb.ts('dve', scb, scb, 1.0, None, ALU.add, r=[ksc_], w=[ksc_])
            mods.append((shb, ksh, scb, ksc_))
        xts = [kb.sb([D]) for _ in range(2)]
        hTs = [bf([8, 128]) for _ in range(2)]
        hms = [bf([D]) for _ in range(2)]
        rs = [kb.sb([64]) for _ in range(2)]
        r2s = [kb.sb([96]) for _ in range(2)]
        Ms = [bf([32]) for _ in range(2)]
        for i, gt in enumerate(tiles):
            b, t, bsel = self.tile_of(gt)
            xt, kxt = xts[i % 2]
            hT, khT = hTs[i % 2]
            self.mt_tile(Xs[b, t * 128:(t + 1) * 128, :], ('X', gt), l, bsel, 3, hT, khT, xt, kxt, (0, 1))
            shb, ksh, scb, ksc_ = mods[bsel]
            hm, khm = hms[i % 2]
            kb.tt('pool', xt, xt, scb, ALU.mult, r=[kxt, ksc_], w=[kxt])
            kb.tt('pool', hm, xt, shb, ALU.add, r=[kxt, ksh], w=[khm])
            kb.dma('sp', HM[i * 128:(i + 1) * 128, :], hm, r=[khm], w=[('HM', i)])
            pb, kpb = kb.bank(2 + i % 2)
            for kc in range(8):
                kb.mm(pb[:, 0:36], hT[:, kc, :], wr[:, kc, :], kc == 0, kc == 7, r=[khT, kwr], w=[kpb])
            R, kR = rs[i % 2]
            Q, kQ = r2s[i % 2]
            L = R[:, 0:36]
            kb.tt('dve', L, pb[:, 0:36], br, ALU.add, r=[kpb, kbr], w=[kR])
            gmax = R[:, 36:37]
            kb.red(gmax, L[:, 0:4], ALU.max, r=[kR], w=[kR])
            ngmax = R[:, 37:38]
            kb.ts('dve', ngmax, gmax, -1.0, None, ALU.mult, r=[kR], w=[kR])
            gsum = R[:, 38:39]
            kb.act(R[:, 40:44], L[:, 0:4], AF.Exp, bias=ngmax, accum=gsum, r=[kR], w=[kR])
            ggate = R[:, 39:40]
            kb.recip(ggate, gsum, r=[kR], w=[kR])
            ohg = R[:, 44:48]
            kb.ts('dve', ohg, L[:, 0:4], gmax, None, ALU.is_equal, r=[kR], w=[kR])
            ein = R[:, 48:56]
            kb.ts('dve', ein, L[:, 4:12], ohg[:, 0:1], None, ALU.mult, r=[kR], w=[kR])
            for g in range(1, 4):
                kb.stt(ein, L[:, 4 + 8 * g:12 + 8 * g], ohg[:, g:g + 1], ein, ALU.mult, ALU.add, r=[kR], w=[kR])
            m1 = R[:, 56:57]
            kb.red(m1, ein, ALU.max, r=[kR], w=[kR])
            oh1 = Q[:, 0:8]
            oh2 = Q[:, 8:16]
            ein2 = Q[:, 16:24]
            kb.ts('dve', oh1, ein, m1, None, ALU.is_equal, r=[kR], w=[kQ])
            kb.stt(ein2, oh1, -1.0e30, ein, ALU.mult, ALU.add, r=[kR, kQ], w=[kQ])
            m2 = R[:, 57:58]
            kb.red(m2, ein2, ALU.max, r=[kQ], w=[kR])
            kb.ts('dve', oh2, ein2, m2, None, ALU.is_equal, r=[kR, kQ], w=[kQ])
            dd = R[:, 58:59]
            kb.tt('dve', dd, m2, m1, ALU.subtract, r=[kR], w=[kR])
            ed = R[:, 59:60]
            kb.act(ed, dd, AF.Exp, r=[kR], w=[kR])
            den = R[:, 60:61]
            kb.ts('dve', den, ed, 1.0, None, ALU.add, r=[kR], w=[kR])
            w1_ = R[:, 61:62]
            kb.recip(w1_, den, r=[kR], w=[kR])
            kPi = kPG + '_%d' % i
            kb.tt('dve', PG[:, i, 2:3], w1_, ggate, ALU.mult, r=[kR], w=[kPi])
            kb.tt('dve', PG[:, i, 3:4], PG[:, i, 2:3], ed, ALU.mult, r=[kR, kPi], w=[kPi])
            kOi = kOH + '_%d' % i
            OHf = Q[:, 32:96]
            for g in range(4):
                kb.ts('dve', OHf[:, 8 * g:8 * g + 8], oh1, ohg[:, g:g + 1], None, ALU.mult, r=[kR, kQ], w=[kQ])
                kb.ts('dve', OHf[:, 32 + 8 * g:40 + 8 * g], oh2, ohg[:, g:g + 1], None, ALU.mult, r=[kR, kQ], w=[kQ])
            kb.cp('dve', OH[:, i, :], OHf, r=[kQ], w=[kOi])
            M, kM = Ms[i % 2]
            kb.tt('dve', M, OHf[:, 0:32], OHf[:, 32:64], ALU.add, r=[kQ], w=[kM])
            pp, kpp = kb.bank(4 + i % 2)
            kb.mm(pp[:, 0:32], su, M, True, True, r=[ksu, kM], w=[kpp])
            kb.mm(pp[:, 32:64], onesb, M, True, True, r=[konesb, kM], w=[kpp])
            pos = R[:, 0:32]
            kb.tt('dve', pos, pp[:, 0:32], carry, ALU.add, r=[kpp, kcarry, kR], w=[kR])
            kb.tt('dve', carry, carry, pp[:, 32:64], ALU.add, r=[kpp, kcarry, kR], w=[kcarry])
            for k in range(2):
                tq = Q[:, 0:32] if k == 0 else R[:, 32:64]
                kk = kQ if k == 0 else kR
                kb.tt('dve', tq, OHf[:, 32 * k:32 * k + 32], pos, ALU.mult, r=[kQ, kR], w=[kk])
                kb.red(PG[:, i, k:k + 1], tq, ALU.add, r=[kk], w=[kPi])
        kb.S.barrier()
        kb.off = base_off
        thr, kthr = kb.sb([68])
        jid, kjid = kb.sb([NBLK])
        pid, kpid = kb.sb([1])
        kb.dma('sp', thr, p['thr68'].partition_broadcast(128), w=[kthr])
        kb.dma('sp', jid, p['jidx'][0:NBLK].partition_broadcast(128), w=[kjid])
        kb.dma('sp', pid, p['pidx'], w=[kpid])
        cmp1, kc1 = kb.sb([32, 68])
        nblk, knb = kb.sb([32])
        pend, kpe = kb.sb([32])
        pst, kps = kb.sb([32])
        kb.tt('dve', cmp1, carry.unsqueeze(2).to_broadcast([128, 32, 68]), thr.unsqueeze(1).to_broadcast([128, 32, 68]), ALU.is_gt,
              r=[kcarry, kthr], w=[kc1])
        kb.red(nblk, cmp1, ALU.add, r=[kc1], w=[knb])
        kb.cp('dve', pend[:, 0:1], nblk[:, 0:1], r=[knb], w=[kpe])
        for e in range(1, 32):
            kb.tt('dve', pend[:, e:e + 1], pend[:, e - 1:e], nblk[:, e:e + 1], ALU.add, r=[kpe, knb], w=[kpe])
        kb.tt('dve', pst, pend, nblk, ALU.subtract, r=[kpe, knb], w=[kps])
        kb.ts('dve', pst, pst, 128.0, None, ALU.mult, r=[kps], w=[kps])
        cmp2, kc2 = kb.sb([NBLK, 32])
        be, kbe = kb.sb([NBLK])
        kb.tt('dve', cmp2, pend.unsqueeze(1).to_broadcast([128, NBLK, 32]), jid.unsqueeze(2).to_broadcast([128, NBLK, 32]), ALU.is_le,
              r=[kpe, kjid], w=[kc2])
        kb.red(be, cmp2, ALU.add, r=[kc2], w=[kbe])
        kb.ts('dve', be, be, 31.0, 128.0, ALU.min, ALU.mult, r=[kbe], w=[kbe])
        kb.ts('dve', be, be, pid[:, 0:1], None, ALU.add, r=[kbe, kpid], w=[kbe])
        kb.cp('dve', WI, be, r=[kbe], w=[kWI])
        tq2, ktq2 = kb.sb([32])
        for i in range(ntl):
            for k in range(2):
                kb.tt('dve', tq2, OH[:, i, 32 * k:32 * k + 32], pst, ALU.mult, r=[kOH + '_%d' % i, kps], w=[ktq2])
                kb.red(DF[:, i, k:k + 1], tq2, ALU.add, r=[ktq2], w=[kDF])
        kb.tt('dve', DF, DF, PG[:, :, 0:2], ALU.add, r=[kDF] + [kPG + '_%d' % i for i in range(ntl)], w=[kDF])
        kb.cp('dve', DI, DF, r=[kDF], w=[kDI])
        kb.S.barrier()
        kb.off = base_off
        zt, kzt = bf([8192])
        kb.memset('pool', zt, 0.0, w=[kzt])
        r0 = 0
        while r0 < NSLOT:
            nr = min(1024, NSLOT - r0)
            kb.dma('sp', XSL[r0:r0 + nr, :].rearrange("(p a) d -> p (a d)", p=128), zt[:, 0:(nr // 128) * D], r=[kzt], w=['XSLz'])
            r0 += nr
        kb.S.barrier()
        hm2 = [bf([D]) for _ in range(3)]
        for i in range(ntl):
            hm, khm = hm2[i % 3]
            kb.dma('sp', hm, HM[i * 128:(i + 1) * 128, :], w=[khm])
            for k in range(2):
                kb.S.dma('pool', lambda e, hm=hm, i=i, k=k: e.indirect_dma_start(
                    out=XSL[:, :], out_offset=bass.IndirectOffsetOnAxis(ap=DI[:, i, k:k + 1], axis=0), in_=hm, in_offset=None),
                    reads=[khm, kDI])
        kb.S.barrier()
        kb.off = base_off
        wfs = [(kb.sb([4096]), kb.sb([4096]), kb.sb([4096])) for _ in range(2)]
        wbs = [(bf([8, 512]), bf([8, 512]), bf([4, D])) for _ in range(2)]
        xbs = [bf([D]) for _ in range(2)]
        xTs = [bf([8, 128]) for _ in range(2)]
        sls = [kb.sb([512]) for _ in range(2)]
        abs_ = [bf([512]) for _ in range(2)]
        aTs = [bf([4, 128]) for _ in range(2)]
        ybs = [kb.sb([D]) for _ in range(2)]
        for j in range(NBLK):
            (w1f, k1f), (w3f, k3f), (w2f, k2f) = wfs[j % 2]
            (w1b, k1b), (w3b, k3b), (w2b, k2b) = wbs[j % 2]
            for (wf, kf, nm) in ((w1f, k1f, 'moe_w1p'), (w3f, k3f, 'moe_w3p'), (w2f, k2f, 'moe_w2p')):
                kb.S.dma('pool', lambda e, wf=wf, nm=nm, j=j: e.indirect_dma_start(
                    out=wf, out_offset=None, in_=p[nm][l][:, :], in_offset=bass.IndirectOffsetOnAxis(ap=WI[:, j:j + 1], axis=0)),
                    reads=[kWI], writes=[kf])
            kb.cp('act', w1b.rearrange("p a b -> p (a b)"), w1f, r=[k1f], w=[k1b])
            kb.cp('dve', w3b.rearrange("p a b -> p (a b)"), w3f, r=[k3f], w=[k3b])
            kb.cp('pool', w2b.rearrange("p a b -> p (a b)"), w2f, r=[k2f], w=[k2b])
            xb, kxb = xbs[j % 2]
            xT, kxT = xTs[j % 2]
            kb.dma('sp', xb, XSL[j * 128:(j + 1) * 128, :], w=[kxb])
            pT, kpT = kb.bank(j % 2, [8, 128], BF16)
            for c in range(8):
                kb.tr(pT[:, c, :], xb[:, c * 128:(c + 1) * 128], self.identb, r=[kxb], w=[kpT])
            kb.cp('dve', xT, pT, r=[kpT], w=[kxT])
            A, kA = kb.bank(2)
            B_, kB = kb.bank(3)
            for c in range(8):
                kb.mm(A, xT[:, c, :], w1b[:, c, :], c == 0, c == 7, r=[kxT, k1b], w=[kA])
            for c in range(8):
                kb.mm(B_, xT[:, c, :], w3b[:, c, :], c == 0, c == 7, r=[kxT, k3b], w=[kB])
            sl, ksl = sls[j % 2]
            ab, kab_ = abs_[j % 2]
            kb.act(sl, A, AF.Silu, r=[kA], w=[ksl])
            kb.tt('dve', ab, sl, B_, ALU.mult, r=[ksl, kB], w=[kab_])
            pT2, kpT2 = kb.bank(4 + j % 2, [8, 128], BF16)
            aT, kaT = aTs[j % 2]
            for c in range(4):
                kb.tr(pT2[:, c, :], ab[:, c * 128:(c + 1) * 128], self.identb, r=[kab_], w=[kpT2])
            kb.cp('act', aT, pT2[:, 0:4, :], r=[kpT2], w=[kaT])
            yb, kyb = ybs[j % 2]
            for nt in range(2):
                O, kO = kb.bank(6 + nt)
                for c in range(4):
                    kb.mm(O, aT[:, c, :], w2b[:, c, nt * 512:(nt + 1) * 512], c == 0, c == 3, r=[kaT, k2b], w=[kO])
                kb.cp('act' if nt == 0 else 'dve', yb[:, nt * 512:(nt + 1) * 512], O, r=[kO], w=[kyb])
            kb.dma('sp', YB[j * 128:(j + 1) * 128, :], yb, r=[kyb])
        kb.S.barrier()
        kb.off = base_off
        lng, kln = kb.sb([D])
        lnb, _ = kb.sb([D])
        kb.dma('sp', lng, p['ln_g'][l, 1].partition_broadcast(128), w=[kln])
        kb.dma('sp', lnb, p['ln_b'][l, 1].partition_broadcast(128), w=[kln])
        gts = []
        for bsel in range(3):
            gt_, kgt = kb.sb([D])
            kb.dma('sp', gt_, self.modd[l, bsel, 5 * D:6 * D].partition_broadcast(128), r=['modd'], w=[kgt])
            gts.append((gt_, kgt))
        x1s = [kb.sb([D]) for _ in range(2)]
        y1s = [kb.sb([D]) for _ in range(2)]
        y2s = [kb.sb([D]) for _ in range(2)]
        outs = [kb.sb([D]) for _ in range(2)]
        sts = [kb.sb([2, 6]) for _ in range(2)]
        mvs = [kb.sb([8]) for _ in range(2)]
        for i, gt in enumerate(tiles):
            b, t, bsel = self.tile_of(gt)
            x1, kx1 = x1s[i % 2]
            y1, ky1 = y1s[i % 2]
            y2, ky2 = y2s[i % 2]
            kb.dma('sp', x1, Xs[b, t * 128:(t + 1) * 128, :], r=[('X', gt)], w=[kx1])
            for (yy, kyy, k) in ((y1, ky1, 0), (y2, ky2, 1)):
                kb.S.dma('pool', lambda e, yy=yy, i=i, k=k: e.indirect_dma_start(
                    out=yy, out_offset=None, in_=YB[:, :], in_offset=bass.IndirectOffsetOnAxis(ap=DI[:, i, k:k + 1], axis=0)),
                    reads=[kDI], writes=[kyy])
            kb.ts('dve', y1, y1, PG[:, i, 2:3], None, ALU.mult, r=[ky1], w=[ky1])
            kb.stt(y1, y2, PG[:, i, 3:4], y1, ALU.mult, ALU.add, r=[ky1, ky2], w=[ky1])
            gtile, kgt = gts[bsel]
            kb.tt('pool', y1, y1, gtile, ALU.mult, r=[ky1, kgt], w=[ky1])
            kb.stt(x1, x1, ALPHA, y1, ALU.mult, ALU.add, r=[kx1, ky1], w=[kx1])
            o, ko = outs[i % 2]
            st6, kst = sts[i % 2]
            mv, kmv = mvs[i % 2]
            self.ln_tile(x1, kx1, o, ko, lng, lnb, kln, st6, kst, mv, kmv)
            dst, kd = Xd(gt)
            kb.dma('sp', dst, o, r=[ko], w=[kd])

    return dict(moe_sparse=moe_sparse)


for _k, _v in _moe_sparse_methods().items():
    setattr(Prog, _k, _v)


FUSED = True


def kernel(**inputs):
    shared = shared_inputs(inputs)
    x = np.asarray(inputs['x'], np.float32)
    ctx = np.asarray(inputs['ctx'], np.float32)
    xin = [np.ascontiguousarray(np.concatenate([ctx[2 * c:2 * c + 2], x[2 * c:2 * c + 2]], axis=1)) for c in range(8)]
    host = [host_inputs(inputs, c) for c in range(8)]
    launches = [list(range(DEPTH))] if FUSED else [[l] for l in range(DEPTH)]
    out = None
    for lays in launches:
        last = lays[-1] == DEPTH - 1
        steps = []
        for l in lays:
            steps += [(l, 'mix'), (l, 'moe')]
        cfg = {'steps': steps, 'debug_x': not last}
        if len(lays) == 1:
            cfg['only_layer'] = lays[0]
        prog = Prog(cfg)
        nc = prog.build()
        sl = {}
        for k, v in shared.items():
            if k not in prog.din:
                continue
            if len(lays) == 1 and k in PER_LAYER:
                sl[k] = np.ascontiguousarray(v[lays[0]:lays[0] + 1])
            elif len(lays) == 1 and k in PER_PAIR:
                sl[k] = np.ascontiguousarray(v[lays[0] // 2:lays[0] // 2 + 1])
            else:
                sl[k] = v
        in_maps = []
        for c in range(8):
            m = dict(sl)
            for k, v in host[c].items():
                if k in prog.din:
                    m[k] = v
            m['xin'] = xin[c]
            in_maps.append(m)
        res = run_bass_kernel_spmd(nc, in_maps, core_ids=list(range(8)))
        if last:
            out = np.concatenate([np.asarray(r['y'], np.float32) for r in res.results], axis=0)
        else:
            xin = [np.ascontiguousarray(np.asarray(r['xres'], np.float32)) for r in res.results]
    return out
```
